# Optimizing a Trainium2 kernel written in Bass

```python
import jax, jax.numpy as jnp
from jax import lax
import numpy as np

D_MODEL = 1024
BATCH = 8
SEQ = 4096
DEPTH = 1

N_MEM = 256
A_HEADS = 8
HEAD_DIM = 64
A_WIDTH = A_HEADS * HEAD_DIM
IDX_HEADS = 4
IDX_DIM = 64
TOPK_MAX = 256
Q_BLOCK = 128
B_GROUPS = 8
B_WIDTH = D_MODEL - A_WIDTH
CONV_B_WIDTH = 31
MIX_WIDTH = A_WIDTH + B_WIDTH
ROPE_THETA = 500000.0
ROT_DIM = HEAD_DIM // 4
CROSS_HEADS = 4
CROSS_HEAD_DIM = D_MODEL // CROSS_HEADS
D_FF = 2816
FFN_CONV_WIDTH = 3
EPS = 1e-6
IN_SPLITS = (A_WIDTH, HEAD_DIM, HEAD_DIM, IDX_HEADS * IDX_DIM, IDX_DIM, IDX_HEADS, 2 * B_WIDTH)
IN_COLS = sum(IN_SPLITS)

kernel_name = 'hybrid_dsa_conformer_parallel_block'


def rmsnorm(x, g):
    xf = x.astype(jnp.float32)
    y = xf * lax.rsqrt(jnp.mean(xf * xf, axis=-1, keepdims=True) + EPS)
    return (y * g.astype(jnp.float32)).astype(x.dtype)


def layernorm(x, g, b):
    xf = x.astype(jnp.float32)
    mu = jnp.mean(xf, axis=-1, keepdims=True)
    var = jnp.mean(jnp.square(xf - mu), axis=-1, keepdims=True)
    y = (xf - mu) * lax.rsqrt(var + EPS)
    return (y * g.astype(jnp.float32) + b.astype(jnp.float32)).astype(x.dtype)


def causal_dwconv(x, w, b):
    width, ch = w.shape
    y = lax.conv_general_dilated(
        x, w.astype(x.dtype)[:, None, :], window_strides=(1,),
        padding=[(width - 1, 0)], dimension_numbers=('NWC', 'WIO', 'NWC'),
        feature_group_count=ch)
    return y + b.astype(x.dtype)


def rope_tables(positions):
    freqs = ROPE_THETA ** (-jnp.arange(0, ROT_DIM, 2, dtype=jnp.float32) / ROT_DIM)
    ang = positions.astype(jnp.float32)[:, :, None, None] * freqs
    return jnp.cos(ang), jnp.sin(ang)


def partial_rope(x, cos, sin):
    xr = x[..., :ROT_DIM].astype(jnp.float32)
    x1, x2 = xr[..., :ROT_DIM // 2], xr[..., ROT_DIM // 2:]
    rot = jnp.concatenate([x1 * cos - x2 * sin, x2 * cos + x1 * sin], axis=-1)
    return jnp.concatenate([rot.astype(x.dtype), x[..., ROT_DIM:]], axis=-1)


def dsa_attention(q, k, v, qi, ki, wi):
    bsz, seq = q.shape[0], q.shape[1]
    nb = seq // Q_BLOCK
    topk = min(TOPK_MAX, seq // 4)
    qi = qi * (IDX_DIM ** -0.5)
    wi = wi * (IDX_HEADS ** -0.5)
    gather = jax.vmap(lambda a, i: a[i])
    key_pos = jnp.arange(seq)

    def block(args):
        bi, qb, qib, wib = args
        t = bi * Q_BLOCK + jnp.arange(Q_BLOCK)
        causal = key_pos[None, :] <= t[:, None]
        rel = jax.nn.relu(jnp.einsum('bqhd,bsd->bqhs', qib, ki).astype(jnp.float32))
        score = jnp.einsum('bqh,bqhs->bqs', wib.astype(jnp.float32), rel)
        score = jnp.where(causal[None], score, -jnp.inf)
        _, sel = lax.top_k(score, topk)
        k_sel = gather(k, sel)
        v_sel = gather(v, sel)
        valid = sel <= t[None, :, None]
        logits = jnp.einsum('bqhd,bqkd->bqhk', qb, k_sel).astype(jnp.float32) * (HEAD_DIM ** -0.5)
        logits = jnp.where(valid[:, :, None, :], logits, -jnp.inf)
        p = jax.nn.softmax(logits, axis=-1).astype(v.dtype)
        return jnp.einsum('bqhk,bqkd->bqhd', p, v_sel)

    to_blocks = lambda a: a.reshape(bsz, nb, Q_BLOCK, *a.shape[2:]).swapaxes(0, 1)
    out = lax.map(block, (jnp.arange(nb), to_blocks(q), to_blocks(qi), to_blocks(wi)))
    return out.swapaxes(0, 1).reshape(bsz, seq, A_WIDTH)


def hybrid_layer(x, mem, cos, sin, norm_mix_g, w_in, w_out, conv_b_w, conv_b_b, ln_b_g, ln_b_b,
                 norm_cross_g, norm_mem_g, w_q_cross, w_k_cross, w_v_cross, w_o_cross,
                 norm_ffn_g, w_gate, w_up, ffn_conv_w, ffn_conv_b, w_down):
    bsz, seq, _ = x.shape
    h = rmsnorm(x, norm_mix_g)
    proj = h @ w_in
    cuts = [int(c) for c in np.cumsum(IN_SPLITS)[:-1]]
    q, k, v, qi, ki, wi, glu = jnp.split(proj, cuts, axis=-1)
    q = partial_rope(q.reshape(bsz, seq, A_HEADS, HEAD_DIM), cos, sin)
    k = partial_rope(k[:, :, None, :], cos, sin)[:, :, 0]
    qi = partial_rope(qi.reshape(bsz, seq, IDX_HEADS, IDX_DIM), cos, sin)
    ki = partial_rope(ki[:, :, None, :], cos, sin)[:, :, 0]
    a_out = dsa_attention(q, k, v, qi, ki, wi)
    ga, gg = jnp.split(glu, 2, axis=-1)
    u = ga * jax.nn.sigmoid(gg)
    u = causal_dwconv(u, conv_b_w, conv_b_b)
    b_out = jax.nn.silu(layernorm(u, ln_b_g, ln_b_b))
    x = x + jnp.concatenate([a_out, b_out], axis=-1) @ w_out
    hq = rmsnorm(x, norm_cross_g)
    m = rmsnorm(mem, norm_mem_g)
    qc = (hq @ w_q_cross).reshape(bsz, seq, CROSS_HEADS, CROSS_HEAD_DIM)
    kc = (m @ w_k_cross).reshape(bsz, -1, CROSS_HEADS, CROSS_HEAD_DIM)
    vc = (m @ w_v_cross).reshape(bsz, -1, CROSS_HEADS, CROSS_HEAD_DIM)
    logits = jnp.einsum('bshd,bnhd->bhsn', qc, kc).astype(jnp.float32) * (CROSS_HEAD_DIM ** -0.5)
    p = jax.nn.softmax(logits, axis=-1).astype(vc.dtype)
    oc = jnp.einsum('bhsn,bnhd->bshd', p, vc).reshape(bsz, seq, D_MODEL)
    x = x + oc @ w_o_cross
    hf = rmsnorm(x, norm_ffn_g)
    g = causal_dwconv(hf @ w_gate, ffn_conv_w, ffn_conv_b)
    x = x + (jax.nn.silu(g) * (hf @ w_up)) @ w_down
    return x


def setup_inputs(seed: int = 0) -> dict:
    key = jax.random.key(seed)
    ks = jax.random.split(key, 24)
    f32 = jnp.float32
    nrm = lambda k, shape, scale: jax.random.normal(k, shape, f32) * scale
    gain = lambda k, shape: 1.0 + 0.02 * jax.random.normal(k, shape, f32)
    L = DEPTH
    offsets = jax.random.randint(ks[2], (BATCH, 1), 0, 2048, dtype=jnp.int32)
    positions = offsets + jnp.arange(SEQ, dtype=jnp.int32)[None, :]
    return {
        'x': nrm(ks[0], (BATCH, SEQ, D_MODEL), 1.0),
        'mem': nrm(ks[1], (BATCH, N_MEM, D_MODEL), 1.0),
        'positions': positions,
        'norm_mix_g': gain(ks[3], (L, D_MODEL)),
        'w_in': nrm(ks[4], (L, D_MODEL, IN_COLS), D_MODEL ** -0.5),
        'w_out': nrm(ks[5], (L, MIX_WIDTH, D_MODEL), MIX_WIDTH ** -0.5),
        'conv_b_w': nrm(ks[6], (L, CONV_B_WIDTH, B_WIDTH), CONV_B_WIDTH ** -0.5),
        'conv_b_b': nrm(ks[7], (L, B_WIDTH), 0.02),
        'ln_b_g': gain(ks[8], (L, B_WIDTH)),
        'ln_b_b': nrm(ks[9], (L, B_WIDTH), 0.02),
        'norm_cross_g': gain(ks[10], (L, D_MODEL)),
        'norm_mem_g': gain(ks[11], (L, D_MODEL)),
        'w_q_cross': nrm(ks[12], (L, D_MODEL, D_MODEL), D_MODEL ** -0.5),
        'w_k_cross': nrm(ks[13], (L, D_MODEL, D_MODEL), D_MODEL ** -0.5),
        'w_v_cross': nrm(ks[14], (L, D_MODEL, D_MODEL), D_MODEL ** -0.5),
        'w_o_cross': nrm(ks[15], (L, D_MODEL, D_MODEL), D_MODEL ** -0.5),
        'norm_ffn_g': gain(ks[16], (L, D_MODEL)),
        'w_gate': nrm(ks[17], (L, D_MODEL, D_FF), D_MODEL ** -0.5),
        'w_up': nrm(ks[18], (L, D_MODEL, D_FF), D_MODEL ** -0.5),
        'ffn_conv_w': nrm(ks[19], (L, FFN_CONV_WIDTH, D_FF), FFN_CONV_WIDTH ** -0.5),
        'ffn_conv_b': nrm(ks[20], (L, D_FF), 0.02),
        'w_down': nrm(ks[21], (L, D_FF, D_MODEL), D_FF ** -0.5),
        'norm_final_g': gain(ks[22], (D_MODEL,)),
    }


def reference(x, mem, positions, norm_mix_g, w_in, w_out, conv_b_w, conv_b_b, ln_b_g, ln_b_b,
              norm_cross_g, norm_mem_g, w_q_cross, w_k_cross, w_v_cross, w_o_cross,
              norm_ffn_g, w_gate, w_up, ffn_conv_w, ffn_conv_b, w_down, norm_final_g):
    cos, sin = rope_tables(positions)
    for l in range(DEPTH):
        x = hybrid_layer(x, mem, cos, sin, norm_mix_g[l], w_in[l], w_out[l], conv_b_w[l], conv_b_b[l],
                         ln_b_g[l], ln_b_b[l], norm_cross_g[l], norm_mem_g[l], w_q_cross[l], w_k_cross[l],
                         w_v_cross[l], w_o_cross[l], norm_ffn_g[l], w_gate[l], w_up[l], ffn_conv_w[l],
                         ffn_conv_b[l], w_down[l])
    return rmsnorm(x, norm_final_g)
```

```python
import math
import contextlib
import numpy as np
import concourse.bass as bass
import concourse.mybir as mybir
from concourse.bass_utils import run_bass_kernel_spmd

F32 = mybir.dt.float32
BF16 = mybir.dt.bfloat16
I32 = mybir.dt.int32
ALU = mybir.AluOpType
AF = mybir.ActivationFunctionType

D = 1024
KC = 8
T = 512
NMEM = 256
DFF = 2816
NF = 22
FG = 11
EPS = 1e-6
TOPK = 256
NEG = -30000.0
NBIS = 14
RING = 5
SLOT = 2048


class Sched:
    ENGS = ("pe", "act", "dve", "pool", "sp")

    def __init__(self, nc, es):
        self.nc = nc
        self.ops = {e: [] for e in self.ENGS}
        self.res = {}
        self.esem = {e: es.enter_context(nc.semaphore("se_" + e)) for e in self.ENGS}
        self.dsem = {}
        self.es = es

    def _new(self):
        return {"w": None, "r": {}}

    def _states(self, buf, key):
        d = self.res.setdefault(buf, {})
        if "*" not in d:
            d["*"] = self._new()
        if key == "*":
            return list(d.values())
        if key not in d:
            d[key] = self._new()
            d[key]["w"] = d["*"]["w"]
            d[key]["r"] = dict(d["*"]["r"])
        return [d[key], d["*"]]

    def _gather(self, eng, reads, writes):
        raw, other = set(), set()
        for (buf, key) in reads:
            for st in self._states(buf, key):
                if st["w"] is not None:
                    raw.add(st["w"])
        for (buf, key) in writes:
            for st in self._states(buf, key):
                if st["w"] is not None:
                    other.add(st["w"])
                other.update(st["r"].values())
        deps = set()
        for dpn in raw:
            if dpn[0] == "E" and dpn[1] == eng and eng in ("pe", "sp"):
                continue
            deps.add(dpn)
        for dpn in other:
            if dpn[0] == "E" and dpn[1] == eng and eng in ("pe", "sp"):
                continue
            deps.add(dpn)
        return deps

    def _record(self, me, rkey, reads, writes):
        for (buf, key) in reads:
            sts = self._states(buf, key)
            if key != "*":
                sts = sts[:1]
            for st in sts:
                st["r"][rkey] = me
        for (buf, key) in writes:
            d = self.res[buf]
            if key == "*":
                for st in d.values():
                    st["w"] = me
                    st["r"] = {}
            else:
                d[key]["w"] = me
                d[key]["r"] = {}

    def op(self, eng, fn, reads=(), writes=()):
        idx = len(self.ops[eng])
        me = ("E", eng, idx)
        deps = self._gather(eng, reads, writes)
        self._record(me, ("E", eng), reads, writes)
        self.ops[eng].append((fn, deps, None))

    def dma(self, eng, fn, sem, reads=(), writes=()):
        if sem not in self.dsem:
            self.dsem[sem] = [self.es.enter_context(self.nc.semaphore("sd_" + sem)), 0]
        self.dsem[sem][1] += 16
        me = ("S", sem, self.dsem[sem][1])
        deps = {dp for dp in self._gather(eng, reads, writes) if not (dp[0] == "S" and dp[1] == sem)}
        self._record(me, ("S", sem), reads, writes)
        self.ops[eng].append((fn, deps, sem))

    def emit(self, block):
        signal = {e: set() for e in self.ENGS}
        for e in self.ENGS:
            for (_, deps, _) in self.ops[e]:
                for dpn in deps:
                    if dpn[0] == "E":
                        signal[dpn[1]].add(dpn[2])
        rank = {}
        for e in self.ENGS:
            r, c = {}, 0
            for i in sorted(signal[e]):
                c += 1
                r[i] = c
            rank[e] = r
        self.nsignal = {e: len(rank[e]) for e in self.ENGS}

        def run(ename, eng):
            known = {}
            for i, (fn, deps, dsem) in enumerate(self.ops[ename]):
                waits = {}
                for dpn in deps:
                    if dpn[0] == "E":
                        k, h, v = ("E", dpn[1]), self.esem[dpn[1]], rank[dpn[1]][dpn[2]]
                    else:
                        k, h, v = ("S", dpn[1]), self.dsem[dpn[1]][0], dpn[2]
                    if v > known.get(k, 0) and v > waits.get(k, (None, 0))[1]:
                        waits[k] = (h, v)
                for k, (h, v) in waits.items():
                    eng.wait_ge(h, v)
                    known[k] = v
                if fn is None:
                    continue
                ins = fn(eng)
                if dsem is not None:
                    ins.then_inc(self.dsem[dsem][0], 16)
                elif i in rank[ename]:
                    ins.then_inc(self.esem[ename], 1)

        block.tensor(lambda e: run("pe", e))
        block.scalar(lambda e: run("act", e))
        block.vector(lambda e: run("dve", e))
        block.gpsimd(lambda e: run("pool", e))
        block.sync(lambda e: run("sp", e))


def cpack_layout():
    off, cur = {}, 0
    for name, n in (("g_mix", 8), ("g_cross", 8), ("g_mem", 8), ("g_ffn", 8), ("g_fin", 8), ("ln_g", 4), ("ln_b", 4),
                    ("cb_b", 4), ("cb_w", 4 * 31), ("f_w", NF * 3), ("f_b", NF), ("freq", 1), ("sgn", 1)):
        off[name] = cur
        cur += n
    return off, cur


def build(S, debug=False):
    NT = S // T
    NQ = S // 128
    nc = bass.Bass("TRN2", target_bir_lowering=False)
    dram = lambda name, shape, dt=F32, kind="ExternalInput": nc.dram_tensor(name, shape, dt, kind=kind).ap()
    x_d = dram("x", [S, D])
    mem_d = dram("mem", [NMEM, D])
    pos_d = dram("pos", [1, S], I32)
    wfm_d = dram("w_fm", [D, 3072])
    wvw_d = dram("w_vw", [D, 68])
    wout_d = dram("w_out", [D, D])
    wq_d = dram("w_q", [D, D])
    wk_d = dram("w_k", [D, D])
    wv_d = dram("w_v", [D, D])
    wo_d = dram("w_o", [D, D])
    wg_d = dram("w_gate", [D, DFF])
    wu_d = dram("w_up", [D, DFF])
    wd_d = dram("w_down", [DFF, D])
    coff, ncp = cpack_layout()
    cp_d = dram("cpack", [128, ncp])
    cm_d = dram("cmats", [128, 128 * 3 + 512])
    out_d = dram("out", [S, D], kind="ExternalOutput")

    es = contextlib.ExitStack()
    with es:
        sb = lambda name, shape, dt=F32: es.enter_context(nc.sbuf_tensor(name, shape, dt))
        sc = Sched(nc, es)
        cp = sb("cp", [128, ncp])
        cmf = sb("cmf", [128, 384])
        cmb = sb("cmb", [128, 128 + 512], BF16)
        ident = cmf[:, 0:128]
        causal = cmf[:, 128:256]
        ones_f = cmf[:, 256:384]
        ones_b = cmb[:, 0:128]
        E4 = cmb[:, 128:640]
        cder = sb("cder", [128, 16 + 124])
        kT2 = sb("kT2", [128, S], BF16)
        kiT2 = sb("kiT2", [128, S], BF16)
        vaug = sb("vaug", [128, NQ, 128], BF16)
        kcT = sb("kcT", [128, 8, NMEM], BF16)
        vc = sb("vc", [128, 2, D], BF16)
        halo = sb("halo", [128, NF, 2])
        uT = sb("uT", [128, 4, 30 + T])
        xT = sb("xT", [128, KC, T])
        hT = sb("hT", [128, KC, T], BF16)
        BB = sb("BB", [128, 8, T], BF16)
        BC = sb("BC", [128, 8, T], BF16)
        prodT = sb("prodT", [128, FG, T], BF16)
        ropeC = sb("ropeC", [128, T])
        ropeS = sb("ropeS", [128, T])
        posi = sb("posi", [128, T], I32)
        scores = sb("scores", [128, max(S, 2048)])
        maskbs = [sb("maskb%d" % i, [128, S], BF16) for i in range(2)]
        pcv = [sb("pcv%d" % i, [128, T], BF16) for i in range(4)]
        rtmp = [sb("rtmp%d" % i, [128, T]) for i in range(2)]
        PT = [sb("PT%d" % i, [128, 1024], BF16) for i in range(2)]
        rec = sb("rec", [128, 1024])
        tmp = [sb("tmp%d" % i, [128, T]) for i in range(6)]
        sqt = [sb("sqt%d" % i, [128, T], BF16) for i in range(2)]
        Gs = [sb("Gs%d" % i, [128, T + 2]) for i in range(2)]
        xins = [sb("xin%d" % i, [128, D]) for i in range(2)]
        osts = [sb("ost%d" % i, [128, D]) for i in range(2)]
        wis = sb("wis", [128, 4, 4])
        bis = sb("bis", [128, 4])
        ring = [sb("ring%d" % i, [128, SLOT], BF16) for i in range(RING)]
        wvw = sb("wvw", [128, KC, 68], BF16)
        ps = [es.enter_context(nc.psum_tensor("ps%d" % i, [128, 1024], F32)) for i in range(4)]
        bank = lambda b: ps[b // 2][:, (b % 2) * 512:(b % 2 + 1) * 512]
        bkey = lambda b: ("ps", b)
        st = {"rr": 0, "pair": 0, "ring": 0, "t": 0, "blk": None, "it": 0, "wide": True}
        NT_ = NT
        NBLK = 12 + 8 + 8 + NF + 16
        wscr = nc.dram_tensor("wscr", [NBLK, 128, SLOT], BF16, kind="Internal").ap()

        def nbank():
            nb_ = 8 if st["wide"] else 6
            b = st["rr"] % nb_
            st["rr"] = (b + 1) % nb_
            return b

        def npair():
            p = st["pair"]
            st["pair"] = (p + 1) % 3
            return p

        def ntmp():
            t = st["t"]
            st["t"] = (t + 1) % 6
            return t

        C = lambda name, i=0: cp[:, coff[name] + i:coff[name] + i + 1]

        sc.dma("sp", lambda e: e.dma_start(out=cp[:], in_=cp_d[:, :]), "c0", writes=[("cp", "*")])
        sc.dma("sp", lambda e: e.dma_start(out=cmf[:], in_=cm_d[:, 0:384]), "c1", writes=[("cmf", "*")])
        sc.dma("pool", lambda e: e.dma_start(out=cmb[:], in_=cm_d[:, 256:896]), "c2", writes=[("cmb", "*")])
        sc.dma("pool", lambda e: e.dma_start(out=wvw[:], in_=wvw_d.rearrange("(kc p) n -> p kc n", p=128)), "c3",
               writes=[("wvw", "*")])
        sc.op("dve", lambda e: e.memset(halo[:], 0.0), writes=[("halo", "*")])
        sc.op("dve", lambda e: e.memset(uT[:], 0.0), writes=[("uT", "*")])
        sc.op("dve", lambda e: e.memset(vaug[:, :, 64:128], 1.0), writes=[("vaug", "*")])
        sc.op("dve", lambda e: e.tensor_scalar(out=cder[:, 0:8], in0=cp[:, coff["ln_g"]:coff["ln_g"] + 8], scalar1=0.5,
                                               scalar2=None, op0=ALU.mult), reads=[("cp", "*")], writes=[("cder", "*")])
        sc.op("dve", lambda e: e.tensor_scalar(out=cder[:, 16:140], in0=cp[:, coff["cb_w"]:coff["cb_w"] + 124],
                                               scalar1=0.5, scalar2=None, op0=ALU.mult),
              reads=[("cp", "*")], writes=[("cder", "*")])
        lng = lambda c: cder[:, c:c + 1]
        lnb = lambda c: cder[:, 4 + c:5 + c]
        cbw = lambda c, j: cder[:, 16 + c * 31 + j:16 + c * 31 + j + 1]

        def wload(dmas):
            s = st["ring"]
            st["ring"] = (s + 1) % RING
            bi = st["blk"]
            if bi is not None:
                st["blk"] = bi + 1
            if bi is None or st["it"] == 0:
                for (vf, src) in dmas:
                    sc.dma("pool", lambda e, vf=vf, src=src, s=s: e.dma_start(out=vf(ring[s]), in_=src), "ring%d" % s,
                           writes=[("ring%d" % s, "*")])
                if bi is not None and NT_ > 1:
                    sc.dma("sp", lambda e, s=s, bi=bi: e.dma_start(out=wscr[bi], in_=ring[s][:, :]), "rst%d" % s,
                           reads=[("ring%d" % s, "*")], writes=[("wscr", bi)])
            else:
                q = "sp" if bi % 2 == 0 else "pool"
                sc.dma(q, lambda e, s=s, bi=bi: e.dma_start(out=ring[s][:, :], in_=wscr[bi]),
                       ("rgs%d" if q == "sp" else "ring%d") % s, reads=[("wscr", bi)], writes=[("ring%d" % s, "*")])
            return s

        def wblock(wd, c0, ncols, k0=0, nk=KC):
            src = wd[k0 * 128:(k0 + nk) * 128, c0:c0 + ncols].rearrange("(kc p) n -> p kc n", p=128)
            return wload([(lambda r: r[:, 0:nk * ncols].rearrange("p (kc n) -> p kc n", kc=nk), src)])

        def rview(s, nk, ncols, off=0, parts=128):
            return ring[s][0:parts, off:off + nk * ncols].rearrange("p (kc n) -> p kc n", kc=nk)

        def transpose_in(src_tile, src_key, dst, dst_key, col0, ncol=128):
            for half in range(2):
                b = nbank()
                for i in range(4):
                    dc = half * 4 + i
                    sc.op("pe", lambda e, b=b, i=i, dc=dc: e.transpose(out=bank(b)[:, i * 128:(i + 1) * 128],
                                                                    in_=src_tile[:, dc * 128:(dc + 1) * 128],
                                                                    identity=ident),
                          reads=[src_key, ("cmf", "*")], writes=[bkey(b)])
                eng = "act" if half == 0 else "dve"
                dv = dst[:, half * 4:half * 4 + 4, col0:col0 + 128]
                sv = bank(b).rearrange("p (a b) -> p a b", a=4)
                if eng == "act":
                    sc.op("act", lambda e, dv=dv, sv=sv: e.activation(out=dv, in_=sv, func=AF.Copy),
                          reads=[bkey(b)], writes=[dst_key])
                else:
                    sc.op("dve", lambda e, dv=dv, sv=sv: e.tensor_copy(out=dv, in_=sv),
                          reads=[bkey(b)], writes=[dst_key])

        def rmsnorm(gname, n, dst=None, inplace=False):
            b = nbank()
            for kc in range(KC):
                q = sqt[kc % 2]
                qk = ("sqt%d" % (kc % 2), "*")
                sc.op("act", lambda e, q=q, kc=kc: e.activation(out=q[:, 0:n], in_=xT[:, kc, 0:n], func=AF.Square),
                      reads=[("xT", kc)], writes=[qk])
                sc.op("pe", lambda e, q=q, kc=kc, b=b: e.matmul(bank(b)[:, 0:n], ones_b, q[:, 0:n], start=(kc == 0),
                                                                 stop=(kc == KC - 1)),
                      reads=[qk, ("cmb", "*")], writes=[bkey(b)])
            t = ntmp()
            tk = ("tmp%d" % t, "*")
            sc.op("dve", lambda e, t=t, b=b: e.tensor_scalar(out=tmp[t][:, 0:n], in0=bank(b)[:, 0:n], scalar1=1.0 / D,
                                                            scalar2=EPS, op0=ALU.mult, op1=ALU.add),
                  reads=[bkey(b)], writes=[tk])
            sc.op("act", lambda e, t=t: e.activation(out=tmp[t][:, 0:n], in_=tmp[t][:, 0:n], func=AF.Ln),
                  reads=[tk], writes=[tk])
            sc.op("act", lambda e, t=t: e.activation(out=tmp[t][:, 0:n], in_=tmp[t][:, 0:n], func=AF.Exp, scale=-0.5),
                  reads=[tk], writes=[tk])
            for kc in range(KC):
                if inplace:
                    sc.op("dve", lambda e, t=t, kc=kc: e.scalar_tensor_tensor(
                        out=xT[:, kc, 0:n], in0=xT[:, kc, 0:n], scalar=C(gname, kc), in1=tmp[t][:, 0:n],
                        op0=ALU.mult, op1=ALU.mult), reads=[("xT", kc), tk, ("cp", "*")], writes=[("xT", kc)])
                else:
                    sc.op("dve", lambda e, t=t, kc=kc: e.scalar_tensor_tensor(
                        out=hT[:, kc, 0:n], in0=xT[:, kc, 0:n], scalar=C(gname, kc), in1=tmp[t][:, 0:n],
                        op0=ALU.mult, op1=ALU.mult), reads=[("xT", kc), tk, ("cp", "*")], writes=[("hT", kc)])

        def proj(s, col, n, b, rhs_fn=None, rkeys=None, nk=KC, ncols=256, off=0):
            wv = rview(s, nk, ncols, off)
            for kc in range(nk):
                rhs = hT[:, kc, 0:n] if rhs_fn is None else rhs_fn(kc)
                rk = ("hT", kc) if rkeys is None else rkeys(kc)
                sc.op("pe", lambda e, wv=wv, kc=kc, rhs=rhs: e.matmul(bank(b)[:, 0:n], wv[:, kc, col:col + 128], rhs,
                                                                      start=(kc == 0), stop=(kc == nk - 1)),
                      reads=[("ring%d" % s, "*"), rk], writes=[bkey(b)])

        for mc in range(2):
            sc.dma("sp", lambda e, mc=mc: e.dma_start(out=xins[mc][:], in_=mem_d[mc * 128:(mc + 1) * 128, :]),
                   "xin%d" % mc, writes=[("xin%d" % mc, "*")])
            transpose_in(xins[mc], ("xin%d" % mc, "*"), xT, ("xT", "*"), mc * 128)
        rmsnorm("g_mem", NMEM)
        for blk in range(4):
            s = wblock(wk_d, blk * 256, 256)
            for cc in range(2):
                b = nbank()
                proj(s, cc * 128, NMEM, b)
                c = blk * 2 + cc
                sc.op("act", lambda e, b=b, c=c: e.activation(out=kcT[:, c, :], in_=bank(b)[:, 0:NMEM], func=AF.Copy),
                      reads=[bkey(b)], writes=[("kcT", c)])
        for blk in range(4):
            s = wblock(wv_d, blk * 256, 256)
            wv_ = rview(s, KC, 256)
            for mc in range(2):
                b = nbank()
                for kc in range(KC):
                    sc.op("pe", lambda e, b=b, kc=kc, mc=mc, wv_=wv_: e.matmul(
                        bank(b)[:, 0:256], hT[:, kc, mc * 128:(mc + 1) * 128], wv_[:, kc, :], start=(kc == 0),
                        stop=(kc == KC - 1)), reads=[("ring%d" % s, "*"), ("hT", kc)], writes=[bkey(b)])
                sc.op("act", lambda e, b=b, mc=mc, blk=blk: e.activation(out=vc[:, mc, blk * 256:(blk + 1) * 256],
                                                                          in_=bank(b)[:, 0:256], func=AF.Copy),
                      reads=[bkey(b)], writes=[("vc", "*")])

        for it in range(NT):
            t0 = it * T
            st["blk"] = 0
            st["it"] = it
            def x_load(itx, s4):
                r0 = itx * T + s4 * 128
                sc.dma("sp", lambda e, r0=r0, s4=s4: e.dma_start(out=xins[s4 % 2][:], in_=x_d[r0:r0 + 128, :]),
                       "xin%d" % (s4 % 2), writes=[("xin%d" % (s4 % 2), "*")])

            for s4 in range(4):
                if it == 0 or s4 >= 2:
                    x_load(it, s4)
                transpose_in(xins[s4 % 2], ("xin%d" % (s4 % 2), "*"), xT, ("xT", "*"), s4 * 128)
            def rope_tables(itx):
                t0r = itx * T
                sc.dma("sp", lambda e, t0=t0r: e.dma_start(out=posi[:], in_=pos_d[0:1, t0:t0 + T].partition_broadcast(128)),
                       "posi", writes=[("posi", "*")])
                ta, tb, tcc = ntmp(), ntmp(), ntmp()
                ka, kb, kcx = ("tmp%d" % ta, "*"), ("tmp%d" % tb, "*"), ("tmp%d" % tcc, "*")
                sc.op("dve", lambda e, ta=ta: e.tensor_copy(out=tmp[ta][:], in_=posi[:]), reads=[("posi", "*")], writes=[ka])
                sc.op("dve", lambda e, ta=ta: e.tensor_scalar(out=tmp[ta][:], in0=tmp[ta][:], scalar1=C("freq"),
                                                              scalar2=None, op0=ALU.mult),
                      reads=[ka, ("cp", "*")], writes=[ka])
                for which, dstt, dkey in ((0, ropeS, ("ropeS", "*")), (1, ropeC, ("ropeC", "*"))):
                    if which == 1:
                        sc.op("dve", lambda e, ta=ta: e.tensor_scalar(out=tmp[ta][:], in0=tmp[ta][:], scalar1=math.pi / 2,
                                                                      scalar2=None, op0=ALU.add), reads=[ka], writes=[ka])
                    sc.op("dve", lambda e, ta=ta: e.tensor_scalar(out=posi[:], in0=tmp[ta][:], scalar1=1.0 / (2 * math.pi),
                                                                  scalar2=None, op0=ALU.mult),
                          reads=[ka], writes=[("posi", "*")])
                    sc.op("dve", lambda e, tb=tb: e.tensor_copy(out=tmp[tb][:], in_=posi[:]), reads=[("posi", "*")],
                          writes=[kb])
                    sc.op("dve", lambda e, ta=ta, tb=tb: e.scalar_tensor_tensor(
                        out=tmp[tb][:], in0=tmp[tb][:], scalar=-2 * math.pi, in1=tmp[ta][:], op0=ALU.mult, op1=ALU.add),
                        reads=[ka, kb], writes=[kb])
                    sc.op("dve", lambda e, tb=tb, tcc=tcc: e.tensor_scalar(out=tmp[tcc][:], in0=tmp[tb][:], scalar1=math.pi,
                                                                          scalar2=2 * math.pi, op0=ALU.is_gt, op1=ALU.mult),
                          reads=[kb], writes=[kcx])
                    sc.op("dve", lambda e, tb=tb, tcc=tcc: e.tensor_tensor(out=tmp[tb][:], in0=tmp[tb][:], in1=tmp[tcc][:],
                                                                          op=ALU.subtract), reads=[kb, kcx], writes=[kb])
                    sc.op("act", lambda e, tb=tb, dstt=dstt: e.activation(out=dstt[:], in_=tmp[tb][:], func=AF.Sin),
                          reads=[kb], writes=[dkey])
                sc.op("dve", lambda e: e.tensor_scalar(out=ropeS[:], in0=ropeS[:], scalar1=C("sgn"), scalar2=None,
                                                       op0=ALU.mult), reads=[("ropeS", "*"), ("cp", "*")],
                      writes=[("ropeS", "*")])

            if it == 0:
                rope_tables(0)
            sc.op("dve", lambda e: e.tensor_copy(out=uT[:, :, 0:30], in_=uT[:, :, T:T + 30]), reads=[("uT", "*")],
                  writes=[("uT", "*")])
            rmsnorm("g_mix", T)
            for blk in range(8):
                s = wblock(wfm_d, blk * 256, 256)
                bA, bB = nbank(), nbank()
                proj(s, 0, T, bA)
                proj(s, 128, T, bB)
                t1, t2 = ntmp(), ntmp()
                k1, k2 = ("tmp%d" % t1, "*"), ("tmp%d" % t2, "*")
                sc.op("dve", lambda e, t1=t1, bA=bA: e.tensor_tensor(out=tmp[t1][:], in0=bank(bA), in1=ropeC[:],
                                                                    op=ALU.mult),
                      reads=[bkey(bA), ("ropeC", "*")], writes=[k1])
                sc.op("dve", lambda e, t2=t2, bB=bB: e.tensor_tensor(out=tmp[t2][:], in0=bank(bB), in1=ropeS[:],
                                                                    op=ALU.mult),
                      reads=[bkey(bB), ("ropeS", "*")], writes=[k2])
                if blk < 6:
                    dv, dk = BB[:, blk, :], ("BB", blk)
                elif blk == 6:
                    dv, dk = kT2[:, t0:t0 + T], ("kT2", it)
                else:
                    dv, dk = kiT2[:, t0:t0 + T], ("kiT2", it)
                sc.op("dve", lambda e, t1=t1, t2=t2, dv=dv: e.tensor_tensor(out=dv, in0=tmp[t1][:], in1=tmp[t2][:],
                                                                           op=ALU.add),
                      reads=[k1, k2], writes=[dk])
            for c in range(4):
                s = wblock(wfm_d, 2048 + c * 256, 256)
                bA, bB = nbank(), nbank()
                proj(s, 0, T, bA)
                proj(s, 128, T, bB)
                t1 = ntmp()
                k1 = ("tmp%d" % t1, "*")
                sc.op("act", lambda e, t1=t1, bB=bB: e.activation(out=tmp[t1][:], in_=bank(bB), func=AF.Tanh, scale=0.5),
                      reads=[bkey(bB)], writes=[k1])
                sc.op("dve", lambda e, t1=t1, bA=bA, c=c: e.scalar_tensor_tensor(
                    out=uT[:, c, 30:30 + T], in0=tmp[t1][:], scalar=1.0, in1=bank(bA), op0=ALU.add, op1=ALU.mult),
                    reads=[k1, bkey(bA)], writes=[("uT", "*")])
            for s4 in range(4):
                b = nbank()
                for kc in range(KC):
                    sc.op("pe", lambda e, b=b, kc=kc, s4=s4: e.matmul(bank(b)[:, 0:68], hT[:, kc, s4 * 128:(s4 + 1) * 128],
                                                                     wvw[:, kc, :], start=(kc == 0), stop=(kc == KC - 1)),
                          reads=[("wvw", "*"), ("hT", kc)], writes=[bkey(b)])
                qi_ = it * 4 + s4
                sc.op("act", lambda e, b=b, qi_=qi_: e.activation(out=vaug[:, qi_, 0:64], in_=bank(b)[:, 0:64],
                                                                  func=AF.Copy), reads=[bkey(b)], writes=[("vaug", qi_), ("vwtok", "*")])
                sc.op("dve", lambda e, b=b, s4=s4: e.tensor_scalar(out=wis[:, s4, :], in0=bank(b)[:, 64:68],
                                                                   scalar1=0.5 * 0.125, scalar2=None, op0=ALU.mult),
                      reads=[bkey(b), ("vwtok", "*")], writes=[("wis", s4)])

            if it + 1 < NT:
                x_load(it + 1, 0)
                x_load(it + 1, 1)

            def dsa_index(s4):
                j = it * 4 + s4
                n = (j + 1) * 128
                nck = (n + 511) // 512
                for ck in range(nck):
                    w = min(512, n - ck * 512)
                    for h in range(4):
                        b = nbank()
                        p0 = (h % 2) * 64
                        sc.op("pe", lambda e, b=b, h=h, p0=p0, ck=ck, w=w, s4=s4: e.matmul(
                            bank(b)[:, 0:w], BB[p0:p0 + 64, 4 + h // 2, s4 * 128:(s4 + 1) * 128],
                            kiT2[p0:p0 + 64, ck * 512:ck * 512 + w], start=True, stop=True),
                            reads=[("BB", 4 + h // 2), ("kiT2", "*")], writes=[bkey(b)])
                        r = rtmp[h % 2]
                        rk = ("rtmp%d" % (h % 2), "*")
                        sc.op("act", lambda e, b=b, r=r, w=w: e.activation(out=r[:, 0:w], in_=bank(b)[:, 0:w],
                                                                           func=AF.Relu), reads=[bkey(b)], writes=[rk])
                        sv = scores[:, ck * 512:ck * 512 + w]
                        if h == 0:
                            sc.op("dve", lambda e, r=r, w=w, sv=sv, s4=s4: e.tensor_scalar(
                                out=sv, in0=r[:, 0:w], scalar1=wis[:, s4, 0:1], scalar2=None, op0=ALU.mult),
                                reads=[rk, ("wis", s4)], writes=[("scores", "*")])
                        else:
                            sc.op("dve", lambda e, r=r, w=w, sv=sv, s4=s4, h=h: e.scalar_tensor_tensor(
                                out=sv, in0=r[:, 0:w], scalar=wis[:, s4, h:h + 1], in1=sv, op0=ALU.mult, op1=ALU.add),
                                reads=[rk, ("wis", s4), ("scores", "*")], writes=[("scores", "*")])
                sc.op("dve", lambda e, j=j: e.tensor_tensor(out=scores[:, j * 128:(j + 1) * 128],
                                                            in0=scores[:, j * 128:(j + 1) * 128], in1=causal,
                                                            op=ALU.add),
                      reads=[("scores", "*"), ("cmf", "*")], writes=[("scores", "*")])

            def dsa_bisect(s4):
                j = it * 4 + s4
                n = (j + 1) * 128
                nA = ((n * 7 // 20) // 128) * 128
                maskb = maskbs[j % 2]
                mk = ("maskb%d" % (j % 2), "*")
                junkA = prodT[:].rearrange("p a b -> p (a b)")
                sc.op("dve", lambda e: e.memset(bis[:, 2:3], 2.0 ** -13), writes=[("bis", 2)])
                for i in range(NBIS):
                    dl = 8.0 / (2 ** i)
                    nxt = dl / 2 if i < NBIS - 1 else dl
                    if nA > 0:
                        sc.op("act", lambda e, nA=nA: e.activation(
                            out=junkA[:, 0:nA], in_=scores[:, 0:nA], func=AF.Sign, bias=bis[:, 2:3], scale=-1.0,
                            accum_out=bis[:, 3:4]),
                            reads=[("scores", "*"), ("bis", 2)], writes=[("prodT", "*"), ("bis", 3)])
                    sc.op("dve", lambda e, n=n, nA=nA, maskb=maskb: e.tensor_scalar(
                        out=maskb[:, nA:n], in0=scores[:, nA:n], scalar1=bis[:, 2:3], scalar2=0.0, op0=ALU.is_gt,
                        op1=ALU.add, accum_out=bis[:, 0:1]),
                        reads=[("scores", "*"), ("bis", 2)], writes=[mk, ("bis", 0)])
                    if nA > 0:
                        sc.op("dve", lambda e: e.scalar_tensor_tensor(
                            out=bis[:, 1:2], in0=bis[:, 0:1], scalar=2.0, in1=bis[:, 3:4], op0=ALU.mult,
                            op1=ALU.subtract), reads=[("bis", 0), ("bis", 3)], writes=[("bis", 1)])
                        sc.op("dve", lambda e, dl=dl, nA=nA: e.tensor_scalar(
                            out=bis[:, 1:2], in0=bis[:, 1:2], scalar1=2.0 * TOPK - 1.0 - nA, scalar2=dl, op0=ALU.is_ge,
                            op1=ALU.mult), reads=[("bis", 1)], writes=[("bis", 1)])
                    else:
                        sc.op("dve", lambda e, dl=dl: e.tensor_scalar(
                            out=bis[:, 1:2], in0=bis[:, 0:1], scalar1=TOPK - 0.5, scalar2=dl, op0=ALU.is_ge,
                            op1=ALU.mult), reads=[("bis", 0)], writes=[("bis", 1)])
                    sc.op("dve", lambda e, nxt=nxt: e.scalar_tensor_tensor(out=bis[:, 2:3], in0=bis[:, 1:2], scalar=-nxt,
                                                                           in1=bis[:, 2:3], op0=ALU.add, op1=ALU.add),
                          reads=[("bis", 1), ("bis", 2)], writes=[("bis", 2)])
                    yield
                sc.op("dve", lambda e, n=n, maskb=maskb: e.tensor_scalar(
                    out=maskb[:, 0:n], in0=scores[:, 0:n], scalar1=bis[:, 2:3], scalar2=NEG, op0=ALU.is_le,
                    op1=ALU.mult), reads=[("scores", "*"), ("bis", 2)], writes=[mk])
                yield

            def dsa_attend(s4):
                j = it * 4 + s4
                maskb = maskbs[j % 2]
                mk = ("maskb%d" % (j % 2), "*")
                def att_L(c):
                    pr = npair()
                    for par in range(2):
                        p0 = par * 64
                        ov = ps[pr][:, par * 512:(par + 1) * 512]
                        sc.op("pe", lambda e, ov=ov, p0=p0, c=c, s4=s4: e.matmul(
                            ov.rearrange("p (a b) -> p a b", a=4), kT2[p0:p0 + 64, c * 128:(c + 1) * 128],
                            BB[p0:p0 + 64, 0:4, s4 * 128:(s4 + 1) * 128], start=True, stop=False),
                            reads=[("kT2", "*"), ("BB", 0), ("BB", 1), ("BB", 2), ("BB", 3)],
                            writes=[bkey(2 * pr + par)])
                    for par in range(2):
                        ov = ps[pr][:, par * 512:(par + 1) * 512]
                        sc.op("pe", lambda e, ov=ov, c=c, maskb=maskb: e.matmul(
                            ov, maskb[:, c * 128:(c + 1) * 128], E4, start=False, stop=True),
                            reads=[mk, ("cmb", "*")], writes=[bkey(2 * pr + par)])
                    pt = PT[c % 2]
                    pk = ("PT%d" % (c % 2), "*")
                    sc.op("act", lambda e, pr=pr, pt=pt: e.activation(out=pt[:], in_=ps[pr][:], func=AF.Exp, scale=0.125),
                          reads=[bkey(2 * pr), bkey(2 * pr + 1)], writes=[pk])

                def att_PV(c):
                    pt = PT[c % 2]
                    pk = ("PT%d" % (c % 2), "*")
                    for par in range(2):
                        sc.op("pe", lambda e, par=par, pt=pt, c=c, j=j: e.matmul(
                            ps[3][:, par * 512:(par + 1) * 512], vaug[:, c, :], pt[:, par * 512:(par + 1) * 512],
                            start=(c == 0), stop=(c == j)), reads=[pk, ("vaug", c)], writes=[bkey(6 + par)])

                att_L(0)
                yield
                for c in range(1, j + 1):
                    att_L(c)
                    att_PV(c - 1)
                    yield
                att_PV(j)
                sc.op("act", lambda e: e.activation(out=rec[64:128, :], in_=ps[3][64:128, :], func=AF.Ln),
                      reads=[bkey(6), bkey(7)], writes=[("rec", "*")])
                sc.op("act", lambda e: e.activation(out=rec[64:128, :], in_=rec[64:128, :], func=AF.Exp, scale=-1.0),
                      reads=[("rec", "*")], writes=[("rec", "*")])
                for par in range(2):
                    sc.op("dve", lambda e, par=par, s4=s4: e.tensor_tensor(
                        out=BC[par * 64:(par + 1) * 64, 0:4, s4 * 128:(s4 + 1) * 128],
                        in0=ps[3][0:64, par * 512:(par + 1) * 512].rearrange("p (a b) -> p a b", a=4),
                        in1=rec[64:128, par * 512:(par + 1) * 512].rearrange("p (a b) -> p a b", a=4), op=ALU.mult),
                        reads=[("rec", "*"), bkey(6 + par)], writes=[("BC", a_) for a_ in range(4)])

            def run_pair(ga, na, gb, nb):
                ia = ib = 0
                da = db = False
                while not (da and db):
                    take_b = (not db) and (da or ib * na <= ia * nb)
                    try:
                        next(gb if take_b else ga)
                    except StopIteration:
                        if take_b:
                            db = True
                        else:
                            da = True
                    else:
                        if take_b:
                            ib += 1
                        else:
                            ia += 1

            st["wide"] = False
            dsa_index(0)
            for _ in dsa_bisect(0):
                pass
            for s4 in range(1, 4):
                dsa_index(s4)
                run_pair(dsa_bisect(s4), NBIS + 1, dsa_attend(s4 - 1), it * 4 + s4 + 1)
            for _ in dsa_attend(3):
                pass
            st["wide"] = True

            yv = scores[:, 0:2048].rearrange("p (c t) -> p c t", c=4)
            bS, bQ = nbank(), nbank()
            for c in range(4):
                b = nbank()
                for jt in range(31):
                    pv = pcv[jt % 4]
                    pk = ("pcv%d" % (jt % 4), "*")
                    if jt % 3 == 0:
                        sc.op("act", lambda e, pv=pv, c=c, jt=jt: e.activation(
                            out=pv[:], in_=uT[:, c, jt:jt + T], func=AF.Copy, scale=cbw(c, jt)),
                            reads=[("uT", "*"), ("cder", "*")], writes=[pk])
                    else:
                        sc.op("dve", lambda e, pv=pv, c=c, jt=jt: e.tensor_scalar(
                            out=pv[:], in0=uT[:, c, jt:jt + T], scalar1=cbw(c, jt), scalar2=None, op0=ALU.mult),
                            reads=[("uT", "*"), ("cder", "*")], writes=[pk])
                    sc.op("pe", lambda e, b=b, pv=pv, jt=jt: e.matmul(bank(b), E4[:, 0:128], pv[:], start=(jt == 0),
                                                                     stop=(jt == 30)),
                          reads=[pk, ("cmb", "*")], writes=[bkey(b)])
                tb = ntmp()
                kb = ("tmp%d" % tb, "*")
                sc.op("act", lambda e, b=b, c=c: e.activation(out=yv[:, c, :], in_=bank(b), func=AF.Identity,
                                                              bias=C("cb_b", c)),
                      reads=[bkey(b), ("cp", "*")], writes=[("scores", "*")])
                sc.op("act", lambda e, tb=tb, c=c: e.activation(out=tmp[tb][:], in_=yv[:, c, :], func=AF.Square),
                      reads=[("scores", "*")], writes=[kb])
                sc.op("pe", lambda e, c=c, bS=bS: e.matmul(bank(bS), ones_f, yv[:, c, :], start=(c == 0), stop=(c == 3)),
                      reads=[("scores", "*"), ("cmf", "*")], writes=[bkey(bS)])
                sc.op("pe", lambda e, c=c, bQ=bQ, tb=tb: e.matmul(bank(bQ), ones_f, tmp[tb][:], start=(c == 0),
                                                                 stop=(c == 3)),
                      reads=[kb, ("cmf", "*")], writes=[bkey(bQ)])
            tm, tv, t2 = ntmp(), ntmp(), ntmp()
            km, kv, k2 = ("tmp%d" % tm, "*"), ("tmp%d" % tv, "*"), ("tmp%d" % t2, "*")
            sc.op("act", lambda e, tm=tm, bS=bS: e.activation(out=tmp[tm][:], in_=bank(bS), func=AF.Copy, scale=1.0 / 512),
                  reads=[bkey(bS)], writes=[km])
            sc.op("dve", lambda e, tm=tm, t2=t2: e.tensor_tensor(out=tmp[t2][:], in0=tmp[tm][:], in1=tmp[tm][:],
                                                                op=ALU.mult), reads=[km], writes=[k2])
            sc.op("dve", lambda e, tv=tv, bQ=bQ: e.tensor_scalar(out=tmp[tv][:], in0=bank(bQ), scalar1=1.0 / 512,
                                                                scalar2=EPS, op0=ALU.mult, op1=ALU.add),
                  reads=[bkey(bQ)], writes=[kv])
            sc.op("dve", lambda e, tv=tv, t2=t2: e.tensor_tensor(out=tmp[tv][:], in0=tmp[tv][:], in1=tmp[t2][:],
                                                                op=ALU.subtract), reads=[kv, k2], writes=[kv])
            sc.op("act", lambda e, tv=tv: e.activation(out=tmp[tv][:], in_=tmp[tv][:], func=AF.Ln),
                  reads=[kv], writes=[kv])
            sc.op("act", lambda e, tv=tv: e.activation(out=tmp[tv][:], in_=tmp[tv][:], func=AF.Exp, scale=-0.5),
                  reads=[kv], writes=[kv])
            for c in range(4):
                ta, tb = ntmp(), ntmp()
                while ta in (tm, tv) or tb in (tm, tv) or ta == tb:
                    ta, tb = ntmp(), ntmp()
                ka, kb = ("tmp%d" % ta, "*"), ("tmp%d" % tb, "*")
                sc.op("dve", lambda e, ta=ta, c=c, tm=tm: e.tensor_tensor(out=tmp[ta][:], in0=yv[:, c, :], in1=tmp[tm][:],
                                                                         op=ALU.subtract),
                      reads=[("scores", "*"), km], writes=[ka])
                sc.op("dve", lambda e, ta=ta, tv=tv: e.tensor_tensor(out=tmp[ta][:], in0=tmp[ta][:], in1=tmp[tv][:],
                                                                    op=ALU.mult), reads=[ka, kv], writes=[ka])
                sc.op("dve", lambda e, ta=ta, c=c: e.tensor_scalar(out=tmp[ta][:], in0=tmp[ta][:], scalar1=lng(c),
                                                                  scalar2=lnb(c), op0=ALU.mult, op1=ALU.add),
                      reads=[ka, ("cder", "*")], writes=[ka])
                sc.op("act", lambda e, ta=ta, tb=tb: e.activation(out=tmp[tb][:], in_=tmp[ta][:], func=AF.Tanh),
                      reads=[ka], writes=[kb])
                sc.op("dve", lambda e, ta=ta, tb=tb, c=c: e.scalar_tensor_tensor(
                    out=BC[:, 4 + c, :], in0=tmp[tb][:], scalar=1.0, in1=tmp[ta][:], op0=ALU.add, op1=ALU.mult),
                    reads=[ka, kb], writes=[("BC", 4 + c)])
            for dc in range(8):
                s = wblock(wout_d, dc * 128, 128)
                b = nbank()
                proj(s, 0, T, b, rhs_fn=lambda kc: BC[:, kc, :], rkeys=lambda kc: ("BC", kc), ncols=128)
                sc.op("dve", lambda e, b=b, dc=dc: e.tensor_tensor(out=xT[:, dc, :], in0=xT[:, dc, :], in1=bank(b),
                                                                  op=ALU.add),
                      reads=[bkey(b), ("xT", dc)], writes=[("xT", dc)])

            rmsnorm("g_cross", T)
            for blk in range(4):
                s = wblock(wq_d, blk * 256, 256)
                for cc in range(2):
                    b = nbank()
                    proj(s, cc * 128, T, b)
                    c = blk * 2 + cc
                    sc.op("act", lambda e, b=b, c=c: e.activation(out=BB[:, c, :], in_=bank(b), func=AF.Copy),
                          reads=[bkey(b)], writes=[("BB", c)])
            for h in range(4):
                pts = []
                for mc in range(2):
                    b = nbank()
                    for kk in range(2):
                        sc.op("pe", lambda e, b=b, h=h, kk=kk, mc=mc: e.matmul(
                            bank(b), kcT[:, 2 * h + kk, mc * 128:(mc + 1) * 128], BB[:, 2 * h + kk, :], start=(kk == 0),
                            stop=(kk == 1)), reads=[("kcT", 2 * h + kk), ("BB", 2 * h + kk)], writes=[bkey(b)])
                    pt = PT[mc][:, 0:512]
                    pk = ("PT%d" % mc, "*")
                    sc.op("act", lambda e, b=b, pt=pt: e.activation(out=pt, in_=bank(b), func=AF.Exp, scale=1.0 / 16),
                          reads=[bkey(b)], writes=[pk])
                    pts.append((pt, pk))
                bD = nbank()
                for mc in range(2):
                    sc.op("pe", lambda e, bD=bD, mc=mc, pt=pts[mc][0]: e.matmul(bank(bD), ones_b, pt, start=(mc == 0),
                                                                              stop=(mc == 1)),
                          reads=[pts[mc][1], ("cmb", "*")], writes=[bkey(bD)])
                tr = ntmp()
                kr = ("tmp%d" % tr, "*")
                sc.op("act", lambda e, tr=tr, bD=bD: e.activation(out=tmp[tr][:], in_=bank(bD), func=AF.Ln),
                      reads=[bkey(bD)], writes=[kr])
                sc.op("act", lambda e, tr=tr: e.activation(out=tmp[tr][:], in_=tmp[tr][:], func=AF.Exp, scale=-1.0),
                      reads=[kr], writes=[kr])
                for dd in range(2):
                    b = nbank()
                    cdx = 2 * h + dd
                    for mc in range(2):
                        sc.op("pe", lambda e, b=b, mc=mc, cdx=cdx, pt=pts[mc][0]: e.matmul(
                            bank(b), vc[:, mc, cdx * 128:(cdx + 1) * 128], pt, start=(mc == 0), stop=(mc == 1)),
                            reads=[pts[mc][1], ("vc", "*")], writes=[bkey(b)])
                    sc.op("dve", lambda e, b=b, cdx=cdx, tr=tr: e.tensor_tensor(out=BC[:, cdx, :], in0=bank(b),
                                                                                in1=tmp[tr][:], op=ALU.mult),
                          reads=[bkey(b), kr], writes=[("BC", cdx)])
            for blk in range(4):
                s = wblock(wo_d, blk * 256, 256)
                for cc in range(2):
                    b = nbank()
                    dc = blk * 2 + cc
                    proj(s, cc * 128, T, b, rhs_fn=lambda kc: BC[:, kc, :], rkeys=lambda kc: ("BC", kc))
                    sc.op("dve", lambda e, b=b, dc=dc: e.tensor_tensor(out=xT[:, dc, :], in0=xT[:, dc, :], in1=bank(b),
                                                                      op=ALU.add),
                          reads=[bkey(b), ("xT", dc)], writes=[("xT", dc)])

            rmsnorm("g_ffn", T)
            if it + 1 < NT:
                rope_tables(it + 1)
            for g in range(NF // FG):
                def ffn_A(fl):
                    f = g * FG + fl
                    srcG = wg_d[:, f * 128:(f + 1) * 128].rearrange("(kc p) n -> p kc n", p=128)
                    srcU = wu_d[:, f * 128:(f + 1) * 128].rearrange("(kc p) n -> p kc n", p=128)
                    s = wload([(lambda r: r[:, 0:1024].rearrange("p (kc n) -> p kc n", kc=8), srcG),
                               (lambda r: r[:, 1024:2048].rearrange("p (kc n) -> p kc n", kc=8), srcU)])
                    bG, bU = nbank(), nbank()
                    proj(s, 0, T, bG, ncols=128, off=0)
                    proj(s, 0, T, bU, ncols=128, off=1024)
                    gs = Gs[f % 2]
                    gk = ("Gs%d" % (f % 2), "*")
                    sc.op("dve", lambda e, gs=gs, f=f: e.tensor_copy(out=gs[:, 0:2], in_=halo[:, f, :]),
                          reads=[("halo", f)], writes=[gk])
                    sc.op("act", lambda e, gs=gs, bG=bG: e.activation(out=gs[:, 2:2 + T], in_=bank(bG), func=AF.Copy),
                          reads=[bkey(bG)], writes=[gk])
                    ta, tb = ntmp(), ntmp()
                    ka, kb = ("tmp%d" % ta, "*"), ("tmp%d" % tb, "*")
                    fw = lambda jj, f=f: cp[:, coff["f_w"] + f * 3 + jj:coff["f_w"] + f * 3 + jj + 1]
                    sc.op("act", lambda e, ta=ta, bG=bG, f=f, fw=fw: e.activation(
                        out=tmp[ta][:], in_=bank(bG), func=AF.Identity, scale=fw(2), bias=C("f_b", f)),
                        reads=[bkey(bG), ("cp", "*")], writes=[ka])
                    sc.op("act", lambda e, gs=gs, f=f: e.activation(out=halo[:, f, :], in_=gs[:, T:T + 2], func=AF.Copy),
                          reads=[gk], writes=[("halo", f)])
                    for jj in (1, 0):
                        sc.op("dve", lambda e, ta=ta, gs=gs, jj=jj, fw=fw: e.scalar_tensor_tensor(
                            out=tmp[ta][:], in0=gs[:, jj:jj + T], scalar=fw(jj), in1=tmp[ta][:], op0=ALU.mult,
                            op1=ALU.add), reads=[gk, ("cp", "*"), ka], writes=[ka])
                    sc.op("act", lambda e, ta=ta, tb=tb: e.activation(out=tmp[tb][:], in_=tmp[ta][:], func=AF.Tanh,
                                                                      scale=0.5), reads=[ka], writes=[kb])
                    return (ta, tb, ka, kb, bU)

                def ffn_B(fl, stt_):
                    ta, tb, ka, kb, bU = stt_
                    sc.op("dve", lambda e, ta=ta, tb=tb: e.scalar_tensor_tensor(
                        out=tmp[tb][:], in0=tmp[tb][:], scalar=1.0, in1=tmp[ta][:], op0=ALU.add, op1=ALU.mult),
                        reads=[ka, kb], writes=[kb])
                    sc.op("dve", lambda e, tb=tb, bU=bU, fl=fl: e.tensor_tensor(out=prodT[:, fl, :], in0=tmp[tb][:],
                                                                               in1=bank(bU), op=ALU.mult),
                          reads=[kb, bkey(bU)], writes=[("prodT", fl)])

                prev = ffn_A(0)
                for fl in range(1, FG):
                    cur = ffn_A(fl)
                    ffn_B(fl - 1, prev)
                    prev = cur
                ffn_B(FG - 1, prev)
                for dc in range(8):
                    src = wd_d[g * FG * 128:(g + 1) * FG * 128, dc * 128:(dc + 1) * 128].rearrange(
                        "(kc p) n -> p kc n", p=128)
                    s = wload([(lambda r: r[:, 0:FG * 128].rearrange("p (kc n) -> p kc n", kc=FG), src)])
                    b = nbank()
                    proj(s, 0, T, b, rhs_fn=lambda kc: prodT[:, kc, :], rkeys=lambda kc: ("prodT", kc), nk=FG, ncols=128)
                    sc.op("dve", lambda e, b=b, dc=dc: e.scalar_tensor_tensor(
                        out=xT[:, dc, :], in0=bank(b), scalar=0.5, in1=xT[:, dc, :], op0=ALU.mult, op1=ALU.add),
                        reads=[bkey(b), ("xT", dc)], writes=[("xT", dc)])

            rmsnorm("g_fin", T, inplace=True)
            for s4 in range(4):
                for half in range(2):
                    b = nbank()
                    for i in range(4):
                        dc = half * 4 + i
                        sc.op("pe", lambda e, b=b, i=i, dc=dc, s4=s4: e.transpose(
                            out=bank(b)[:, i * 128:(i + 1) * 128], in_=xT[:, dc, s4 * 128:(s4 + 1) * 128],
                            identity=ident), reads=[("xT", dc), ("cmf", "*")], writes=[bkey(b)])
                    if half == 0:
                        sc.op("act", lambda e, b=b, s4=s4: e.activation(out=osts[s4 % 2][:, 0:512], in_=bank(b),
                                                                        func=AF.Copy),
                              reads=[bkey(b)], writes=[("ost%d" % (s4 % 2), 0)])
                    else:
                        sc.op("dve", lambda e, b=b, s4=s4: e.tensor_copy(out=osts[s4 % 2][:, 512:1024], in_=bank(b)),
                              reads=[bkey(b)], writes=[("ost%d" % (s4 % 2), 1)])
                sc.dma("sp", lambda e, r0=t0 + s4 * 128, s4=s4: e.dma_start(out=out_d[r0:r0 + 128, :],
                                                                            in_=osts[s4 % 2][:]),
                       "ost%d" % (s4 % 2), reads=[("ost%d" % (s4 % 2), "*")])
        sc.op("sp", None, writes=[("ost0", "*"), ("ost1", "*")])
        block = es.enter_context(nc.Block())
        sc.emit(block)
    return nc


def _swap_cols(n_heads):
    idx = []
    for h in range(n_heads):
        base = h * 64
        idx += [base + 8 + i for i in range(8)] + [base + i for i in range(8)] + [base + i for i in range(16, 64)]
    return np.array(idx)


def host_layout(inputs):
    f = lambda k: np.asarray(inputs[k], dtype=np.float32)
    w_in = f("w_in")[0]
    q, k, v, qi, ki, wi, glu = np.split(w_in, np.cumsum([512, 64, 64, 256, 64, 4, 1024])[:-1], axis=1)
    base = np.concatenate([q, qi, k, k, ki, ki], axis=1)
    sw = base[:, _swap_cols(16)]
    ga, gg = glu[:, :512], glu[:, 512:]
    blocks = []
    for c in range(8):
        blocks += [base[:, c * 128:(c + 1) * 128], sw[:, c * 128:(c + 1) * 128]]
    for c in range(4):
        blocks += [ga[:, c * 128:(c + 1) * 128], gg[:, c * 128:(c + 1) * 128]]
    w_fm = np.ascontiguousarray(np.concatenate(blocks, axis=1))
    w_vw = np.ascontiguousarray(np.concatenate([v, wi], axis=1))
    coff, ncp = cpack_layout()
    cpk = np.zeros((128, ncp), np.float32)
    col = lambda vec, n: vec.reshape(n, 128).T
    cpk[:, coff["g_mix"]:coff["g_mix"] + 8] = col(f("norm_mix_g")[0], 8)
    cpk[:, coff["g_cross"]:coff["g_cross"] + 8] = col(f("norm_cross_g")[0], 8)
    cpk[:, coff["g_mem"]:coff["g_mem"] + 8] = col(f("norm_mem_g")[0], 8)
    cpk[:, coff["g_ffn"]:coff["g_ffn"] + 8] = col(f("norm_ffn_g")[0], 8)
    cpk[:, coff["g_fin"]:coff["g_fin"] + 8] = col(f("norm_final_g"), 8)
    cpk[:, coff["ln_g"]:coff["ln_g"] + 4] = col(f("ln_b_g")[0], 4)
    cpk[:, coff["ln_b"]:coff["ln_b"] + 4] = col(f("ln_b_b")[0], 4)
    cpk[:, coff["cb_b"]:coff["cb_b"] + 4] = col(f("conv_b_b")[0], 4)
    cw = f("conv_b_w")[0]
    cpk[:, coff["cb_w"]:coff["cb_w"] + 124] = cw.reshape(31, 4, 128).transpose(2, 1, 0).reshape(128, 124)
    fw = f("ffn_conv_w")[0]
    cpk[:, coff["f_w"]:coff["f_w"] + NF * 3] = fw.reshape(3, NF, 128).transpose(2, 1, 0).reshape(128, NF * 3)
    cpk[:, coff["f_b"]:coff["f_b"] + NF] = col(f("ffn_conv_b")[0], NF)
    p = np.arange(128) % 64
    freqs = (np.float32(500000.0) ** (-np.arange(0, 16, 2, dtype=np.float32) / np.float32(16))).astype(np.float32)
    cpk[:, coff["freq"]] = np.where(p < 16, freqs[p % 8], 0.0)
    cpk[:, coff["sgn"]] = np.where(p < 8, -1.0, np.where(p < 16, 1.0, 0.0))
    cm = np.zeros((128, 896), np.float32)
    cm[:, 0:128] = np.eye(128)
    qq, kk = np.meshgrid(np.arange(128), np.arange(128), indexing="ij")
    cm[:, 128:256] = np.where(kk <= qq, 0.0, -1e30)
    cm[:, 256:384] = 1.0
    cm[:, 384:896] = np.tile(np.eye(128, dtype=np.float32), (1, 4))
    shared = {
        "w_fm": w_fm, "w_vw": w_vw, "w_out": f("w_out")[0], "w_q": f("w_q_cross")[0], "w_k": f("w_k_cross")[0],
        "w_v": f("w_v_cross")[0], "w_o": f("w_o_cross")[0], "w_gate": f("w_gate")[0], "w_up": f("w_up")[0],
        "w_down": f("w_down")[0], "cpack": cpk, "cmats": cm,
    }
    return shared


_NC_CACHE = {}


def kernel(**inputs):
    x = np.asarray(inputs["x"], dtype=np.float32)
    mem = np.asarray(inputs["mem"], dtype=np.float32)
    pos = np.asarray(inputs["positions"], dtype=np.int32)
    B, S, _ = x.shape
    shared = host_layout(inputs)
    if S not in _NC_CACHE:
        _NC_CACHE[S] = build(S)
    nc = _NC_CACHE[S]
    in_maps = []
    for b in range(B):
        m = dict(shared)
        m["x"] = np.ascontiguousarray(x[b])
        m["mem"] = np.ascontiguousarray(mem[b])
        m["pos"] = np.ascontiguousarray(pos[b][None, :])
        in_maps.append(m)
    res = run_bass_kernel_spmd(nc, in_maps, core_ids=list(range(B)))
    return np.stack([np.asarray(r["out"], dtype=np.float32) for r in res.results], axis=0)
```

```python
import math
import contextlib
import numpy as np
import concourse.bass as bass
import concourse.mybir as mybir
from concourse.bass_utils import run_bass_kernel_spmd

F32 = mybir.dt.float32
BF16 = mybir.dt.bfloat16
I32 = mybir.dt.int32
ALU = mybir.AluOpType
AF = mybir.ActivationFunctionType

D = 1024
KC = 8
T = 512
NMEM = 256
DFF = 2816
NF = 22
FG = 11
EPS = 1e-6
TOPK = 256
NEG = -30000.0
NBIS = 14
RING = 5
SLOT = 2048


class Sched:
    ENGS = ("pe", "act", "dve", "pool", "sp")

    def __init__(self, nc, es):
        self.nc = nc
        self.ops = {e: [] for e in self.ENGS}
        self.res = {}
        self.esem = {e: es.enter_context(nc.semaphore("se_" + e)) for e in self.ENGS}
        self.dsem = {}
        self.es = es

    def _new(self):
        return {"w": None, "r": {}}

    def _states(self, buf, key):
        d = self.res.setdefault(buf, {})
        if "*" not in d:
            d["*"] = self._new()
        if key == "*":
            return list(d.values())
        if key not in d:
            d[key] = self._new()
            d[key]["w"] = d["*"]["w"]
            d[key]["r"] = dict(d["*"]["r"])
        return [d[key], d["*"]]

    def _gather(self, eng, reads, writes):
        raw, other = set(), set()
        for (buf, key) in reads:
            for st in self._states(buf, key):
                if st["w"] is not None:
                    raw.add(st["w"])
        for (buf, key) in writes:
            for st in self._states(buf, key):
                if st["w"] is not None:
                    other.add(st["w"])
                other.update(st["r"].values())
        deps = set()
        for dpn in raw:
            if dpn[0] == "E" and dpn[1] == eng and eng in ("pe", "sp"):
                continue
            deps.add(dpn)
        for dpn in other:
            if dpn[0] == "E" and dpn[1] == eng and eng in ("pe", "sp"):
                continue
            deps.add(dpn)
        return deps

    def _record(self, me, rkey, reads, writes):
        for (buf, key) in reads:
            sts = self._states(buf, key)
            if key != "*":
                sts = sts[:1]
            for st in sts:
                st["r"][rkey] = me
        for (buf, key) in writes:
            d = self.res[buf]
            if key == "*":
                for st in d.values():
                    st["w"] = me
                    st["r"] = {}
            else:
                d[key]["w"] = me
                d[key]["r"] = {}

    def op(self, eng, fn, reads=(), writes=()):
        idx = len(self.ops[eng])
        me = ("E", eng, idx)
        deps = self._gather(eng, reads, writes)
        self._record(me, ("E", eng), reads, writes)
        self.ops[eng].append((fn, deps, None))

    def dma(self, eng, fn, sem, reads=(), writes=()):
        if sem not in self.dsem:
            self.dsem[sem] = [self.es.enter_context(self.nc.semaphore("sd_" + sem)), 0]
        self.dsem[sem][1] += 16
        me = ("S", sem, self.dsem[sem][1])
        deps = {dp for dp in self._gather(eng, reads, writes) if not (dp[0] == "S" and dp[1] == sem)}
        self._record(me, ("S", sem), reads, writes)
        self.ops[eng].append((fn, deps, sem))

    def emit(self, block):
        signal = {e: set() for e in self.ENGS}
        for e in self.ENGS:
            for (_, deps, _) in self.ops[e]:
                for dpn in deps:
                    if dpn[0] == "E":
                        signal[dpn[1]].add(dpn[2])
        rank = {}
        for e in self.ENGS:
            r, c = {}, 0
            for i in sorted(signal[e]):
                c += 1
                r[i] = c
            rank[e] = r
        self.nsignal = {e: len(rank[e]) for e in self.ENGS}

        def run(ename, eng):
            known = {}
            for i, (fn, deps, dsem) in enumerate(self.ops[ename]):
                waits = {}
                for dpn in deps:
                    if dpn[0] == "E":
                        k, h, v = ("E", dpn[1]), self.esem[dpn[1]], rank[dpn[1]][dpn[2]]
                    else:
                        k, h, v = ("S", dpn[1]), self.dsem[dpn[1]][0], dpn[2]
                    if v > known.get(k, 0) and v > waits.get(k, (None, 0))[1]:
                        waits[k] = (h, v)
                for k, (h, v) in waits.items():
                    eng.wait_ge(h, v)
                    known[k] = v
                if fn is None:
                    continue
                ins = fn(eng)
                if dsem is not None:
                    ins.then_inc(self.dsem[dsem][0], 16)
                elif i in rank[ename]:
                    ins.then_inc(self.esem[ename], 1)

        block.tensor(lambda e: run("pe", e))
        block.scalar(lambda e: run("act", e))
        block.vector(lambda e: run("dve", e))
        block.gpsimd(lambda e: run("pool", e))
        block.sync(lambda e: run("sp", e))


def cpack_layout():
    off, cur = {}, 0
    for name, n in (("g_mix", 8), ("g_cross", 8), ("g_mem", 8), ("g_ffn", 8), ("g_fin", 8), ("ln_g", 4), ("ln_b", 4),
                    ("cb_b", 4), ("cb_w", 4 * 31), ("f_w", NF * 3), ("f_b", NF), ("freq", 1), ("sgn", 1)):
        off[name] = cur
        cur += n
    return off, cur


def build(S, debug=False):
    NT = S // T
    NQ = S // 128
    nc = bass.Bass("TRN2", target_bir_lowering=False)
    dram = lambda name, shape, dt=F32, kind="ExternalInput": nc.dram_tensor(name, shape, dt, kind=kind).ap()
    x_d = dram("x", [S, D])
    mem_d = dram("mem", [NMEM, D])
    pos_d = dram("pos", [1, S], I32)
    wfm_d = dram("w_fm", [D, 3072])
    wvw_d = dram("w_vw", [D, 68])
    wout_d = dram("w_out", [D, D])
    wq_d = dram("w_q", [D, D])
    wk_d = dram("w_k", [D, D])
    wv_d = dram("w_v", [D, D])
    wo_d = dram("w_o", [D, D])
    wg_d = dram("w_gate", [D, DFF])
    wu_d = dram("w_up", [D, DFF])
    wd_d = dram("w_down", [DFF, D])
    coff, ncp = cpack_layout()
    cp_d = dram("cpack", [128, ncp])
    cm_d = dram("cmats", [128, 128 * 3 + 512])
    out_d = dram("out", [S, D], kind="ExternalOutput")

    es = contextlib.ExitStack()
    with es:
        sb = lambda name, shape, dt=F32: es.enter_context(nc.sbuf_tensor(name, shape, dt))
        sc = Sched(nc, es)
        cp = sb("cp", [128, ncp])
        cmf = sb("cmf", [128, 384])
        cmb = sb("cmb", [128, 128 + 512], BF16)
        ident = cmf[:, 0:128]
        causal = cmf[:, 128:256]
        ones_f = cmf[:, 256:384]
        ones_b = cmb[:, 0:128]
        E4 = cmb[:, 128:640]
        cder = sb("cder", [128, 16 + 124])
        kT2 = sb("kT2", [128, S], BF16)
        kiT2 = sb("kiT2", [128, S], BF16)
        vaug = sb("vaug", [128, NQ, 128], BF16)
        kcT = sb("kcT", [128, 8, NMEM], BF16)
        vc = sb("vc", [128, 2, D], BF16)
        halo = sb("halo", [128, NF, 2])
        uT = sb("uT", [128, 4, 30 + T])
        xT = sb("xT", [128, KC, T])
        hT = sb("hT", [128, KC, T], BF16)
        BB = sb("BB", [128, 8, T], BF16)
        BC = sb("BC", [128, 8, T], BF16)
        prodT = sb("prodT", [128, FG, T], BF16)
        ropeC = sb("ropeC", [128, T])
        ropeS = sb("ropeS", [128, T])
        posi = sb("posi", [128, T], I32)
        scores = sb("scores", [128, max(S, 2048)])
        maskbs = [sb("maskb%d" % i, [128, S], BF16) for i in range(2)]
        pcv = [sb("pcv%d" % i, [128, T], BF16) for i in range(4)]
        rtmp = [sb("rtmp%d" % i, [128, T]) for i in range(2)]
        PT = [sb("PT%d" % i, [128, 1024], BF16) for i in range(2)]
        rec = sb("rec", [128, 1024])
        tmp = [sb("tmp%d" % i, [128, T]) for i in range(6)]
        sqt = [sb("sqt%d" % i, [128, T], BF16) for i in range(2)]
        Gs = [sb("Gs%d" % i, [128, T + 2]) for i in range(2)]
        xins = [sb("xin%d" % i, [128, D]) for i in range(2)]
        osts = [sb("ost%d" % i, [128, D]) for i in range(2)]
        wis = sb("wis", [128, 4, 4])
        bis = sb("bis", [128, 4])
        ring = [sb("ring%d" % i, [128, SLOT], BF16) for i in range(RING)]
        wvw = sb("wvw", [128, KC, 68], BF16)
        ps = [es.enter_context(nc.psum_tensor("ps%d" % i, [128, 1024], F32)) for i in range(4)]
        bank = lambda b: ps[b // 2][:, (b % 2) * 512:(b % 2 + 1) * 512]
        bkey = lambda b: ("ps", b)
        st = {"rr": 0, "pair": 0, "ring": 0, "t": 0, "blk": None, "it": 0, "wide": True}
        NT_ = NT
        NBLK = 12 + 8 + 8 + NF + 16
        wscr = nc.dram_tensor("wscr", [NBLK, 128, SLOT], BF16, kind="Internal").ap()

        def nbank():
            nb_ = 8 if st["wide"] else 6
            b = st["rr"] % nb_
            st["rr"] = (b + 1) % nb_
            return b

        def npair():
            p = st["pair"]
            st["pair"] = (p + 1) % 3
            return p

        def ntmp():
            t = st["t"]
            st["t"] = (t + 1) % 6
            return t

        C = lambda name, i=0: cp[:, coff[name] + i:coff[name] + i + 1]

        sc.dma("sp", lambda e: e.dma_start(out=cp[:], in_=cp_d[:, :]), "c0", writes=[("cp", "*")])
        sc.dma("sp", lambda e: e.dma_start(out=cmf[:], in_=cm_d[:, 0:384]), "c1", writes=[("cmf", "*")])
        sc.dma("pool", lambda e: e.dma_start(out=cmb[:], in_=cm_d[:, 256:896]), "c2", writes=[("cmb", "*")])
        sc.dma("pool", lambda e: e.dma_start(out=wvw[:], in_=wvw_d.rearrange("(kc p) n -> p kc n", p=128)), "c3",
               writes=[("wvw", "*")])
        sc.op("dve", lambda e: e.memset(halo[:], 0.0), writes=[("halo", "*")])
        sc.op("dve", lambda e: e.memset(uT[:], 0.0), writes=[("uT", "*")])
        sc.op("dve", lambda e: e.memset(vaug[:, :, 64:128], 1.0), writes=[("vaug", "*")])
        sc.op("dve", lambda e: e.tensor_scalar(out=cder[:, 0:8], in0=cp[:, coff["ln_g"]:coff["ln_g"] + 8], scalar1=0.5,
                                               scalar2=None, op0=ALU.mult), reads=[("cp", "*")], writes=[("cder", "*")])
        sc.op("dve", lambda e: e.tensor_scalar(out=cder[:, 16:140], in0=cp[:, coff["cb_w"]:coff["cb_w"] + 124],
                                               scalar1=0.5, scalar2=None, op0=ALU.mult),
              reads=[("cp", "*")], writes=[("cder", "*")])
        lng = lambda c: cder[:, c:c + 1]
        lnb = lambda c: cder[:, 4 + c:5 + c]
        cbw = lambda c, j: cder[:, 16 + c * 31 + j:16 + c * 31 + j + 1]

        def wload(dmas):
            s = st["ring"]
            st["ring"] = (s + 1) % RING
            bi = st["blk"]
            if bi is not None:
                st["blk"] = bi + 1
            if bi is None or st["it"] == 0:
                for (vf, src) in dmas:
                    sc.dma("pool", lambda e, vf=vf, src=src, s=s: e.dma_start(out=vf(ring[s]), in_=src), "ring%d" % s,
                           writes=[("ring%d" % s, "*")])
                if bi is not None and NT_ > 1:
                    sc.dma("sp", lambda e, s=s, bi=bi: e.dma_start(out=wscr[bi], in_=ring[s][:, :]), "rst%d" % s,
                           reads=[("ring%d" % s, "*")], writes=[("wscr", bi)])
            else:
                q = "sp" if bi % 2 == 0 else "pool"
                sc.dma(q, lambda e, s=s, bi=bi: e.dma_start(out=ring[s][:, :], in_=wscr[bi]),
                       ("rgs%d" if q == "sp" else "ring%d") % s, reads=[("wscr", bi)], writes=[("ring%d" % s, "*")])
            return s

        def wblock(wd, c0, ncols, k0=0, nk=KC):
            src = wd[k0 * 128:(k0 + nk) * 128, c0:c0 + ncols].rearrange("(kc p) n -> p kc n", p=128)
            return wload([(lambda r: r[:, 0:nk * ncols].rearrange("p (kc n) -> p kc n", kc=nk), src)])

        def rview(s, nk, ncols, off=0, parts=128):
            return ring[s][0:parts, off:off + nk * ncols].rearrange("p (kc n) -> p kc n", kc=nk)

        def transpose_in(src_tile, src_key, dst, dst_key, col0, ncol=128):
            for half in range(2):
                b = nbank()
                for i in range(4):
                    dc = half * 4 + i
                    sc.op("pe", lambda e, b=b, i=i, dc=dc: e.transpose(out=bank(b)[:, i * 128:(i + 1) * 128],
                                                                    in_=src_tile[:, dc * 128:(dc + 1) * 128],
                                                                    identity=ident),
                          reads=[src_key, ("cmf", "*")], writes=[bkey(b)])
                eng = "act" if half == 0 else "dve"
                dv = dst[:, half * 4:half * 4 + 4, col0:col0 + 128]
                sv = bank(b).rearrange("p (a b) -> p a b", a=4)
                if eng == "act":
                    sc.op("act", lambda e, dv=dv, sv=sv: e.activation(out=dv, in_=sv, func=AF.Copy),
                          reads=[bkey(b)], writes=[dst_key])
                else:
                    sc.op("dve", lambda e, dv=dv, sv=sv: e.tensor_copy(out=dv, in_=sv),
                          reads=[bkey(b)], writes=[dst_key])

        def rmsnorm(gname, n, dst=None, inplace=False):
            b = nbank()
            for kc in range(KC):
                q = sqt[kc % 2]
                qk = ("sqt%d" % (kc % 2), "*")
                sc.op("act", lambda e, q=q, kc=kc: e.activation(out=q[:, 0:n], in_=xT[:, kc, 0:n], func=AF.Square),
                      reads=[("xT", kc)], writes=[qk])
                sc.op("pe", lambda e, q=q, kc=kc, b=b: e.matmul(bank(b)[:, 0:n], ones_b, q[:, 0:n], start=(kc == 0),
                                                                 stop=(kc == KC - 1)),
                      reads=[qk, ("cmb", "*")], writes=[bkey(b)])
            t = ntmp()
            tk = ("tmp%d" % t, "*")
            sc.op("dve", lambda e, t=t, b=b: e.tensor_scalar(out=tmp[t][:, 0:n], in0=bank(b)[:, 0:n], scalar1=1.0 / D,
                                                            scalar2=EPS, op0=ALU.mult, op1=ALU.add),
                  reads=[bkey(b)], writes=[tk])
            sc.op("act", lambda e, t=t: e.activation(out=tmp[t][:, 0:n], in_=tmp[t][:, 0:n], func=AF.Ln),
                  reads=[tk], writes=[tk])
            sc.op("act", lambda e, t=t: e.activation(out=tmp[t][:, 0:n], in_=tmp[t][:, 0:n], func=AF.Exp, scale=-0.5),
                  reads=[tk], writes=[tk])
            for kc in range(KC):
                if inplace:
                    sc.op("dve", lambda e, t=t, kc=kc: e.scalar_tensor_tensor(
                        out=xT[:, kc, 0:n], in0=xT[:, kc, 0:n], scalar=C(gname, kc), in1=tmp[t][:, 0:n],
                        op0=ALU.mult, op1=ALU.mult), reads=[("xT", kc), tk, ("cp", "*")], writes=[("xT", kc)])
                else:
                    sc.op("dve", lambda e, t=t, kc=kc: e.scalar_tensor_tensor(
                        out=hT[:, kc, 0:n], in0=xT[:, kc, 0:n], scalar=C(gname, kc), in1=tmp[t][:, 0:n],
                        op0=ALU.mult, op1=ALU.mult), reads=[("xT", kc), tk, ("cp", "*")], writes=[("hT", kc)])

        def proj(s, col, n, b, rhs_fn=None, rkeys=None, nk=KC, ncols=256, off=0):
            wv = rview(s, nk, ncols, off)
            for kc in range(nk):
                rhs = hT[:, kc, 0:n] if rhs_fn is None else rhs_fn(kc)
                rk = ("hT", kc) if rkeys is None else rkeys(kc)
                sc.op("pe", lambda e, wv=wv, kc=kc, rhs=rhs: e.matmul(bank(b)[:, 0:n], wv[:, kc, col:col + 128], rhs,
                                                                      start=(kc == 0), stop=(kc == nk - 1)),
                      reads=[("ring%d" % s, "*"), rk], writes=[bkey(b)])

        for mc in range(2):
            sc.dma("sp", lambda e, mc=mc: e.dma_start(out=xins[mc][:], in_=mem_d[mc * 128:(mc + 1) * 128, :]),
                   "xin%d" % mc, writes=[("xin%d" % mc, "*")])
            transpose_in(xins[mc], ("xin%d" % mc, "*"), xT, ("xT", "*"), mc * 128)
        rmsnorm("g_mem", NMEM)
        for blk in range(4):
            s = wblock(wk_d, blk * 256, 256)
            for cc in range(2):
                b = nbank()
                proj(s, cc * 128, NMEM, b)
                c = blk * 2 + cc
                sc.op("act", lambda e, b=b, c=c: e.activation(out=kcT[:, c, :], in_=bank(b)[:, 0:NMEM], func=AF.Copy),
                      reads=[bkey(b)], writes=[("kcT", c)])
        for blk in range(4):
            s = wblock(wv_d, blk * 256, 256)
            wv_ = rview(s, KC, 256)
            for mc in range(2):
                b = nbank()
                for kc in range(KC):
                    sc.op("pe", lambda e, b=b, kc=kc, mc=mc, wv_=wv_: e.matmul(
                        bank(b)[:, 0:256], hT[:, kc, mc * 128:(mc + 1) * 128], wv_[:, kc, :], start=(kc == 0),
                        stop=(kc == KC - 1)), reads=[("ring%d" % s, "*"), ("hT", kc)], writes=[bkey(b)])
                sc.op("act", lambda e, b=b, mc=mc, blk=blk: e.activation(out=vc[:, mc, blk * 256:(blk + 1) * 256],
                                                                          in_=bank(b)[:, 0:256], func=AF.Copy),
                      reads=[bkey(b)], writes=[("vc", "*")])

        for it in range(NT):
            t0 = it * T
            st["blk"] = 0
            st["it"] = it
            def x_load(itx, s4):
                r0 = itx * T + s4 * 128
                sc.dma("sp", lambda e, r0=r0, s4=s4: e.dma_start(out=xins[s4 % 2][:], in_=x_d[r0:r0 + 128, :]),
                       "xin%d" % (s4 % 2), writes=[("xin%d" % (s4 % 2), "*")])

            for s4 in range(4):
                if it == 0 or s4 >= 2:
                    x_load(it, s4)
                transpose_in(xins[s4 % 2], ("xin%d" % (s4 % 2), "*"), xT, ("xT", "*"), s4 * 128)
            def rope_tables(itx):
                t0r = itx * T
                sc.dma("sp", lambda e, t0=t0r: e.dma_start(out=posi[:], in_=pos_d[0:1, t0:t0 + T].partition_broadcast(128)),
                       "posi", writes=[("posi", "*")])
                ta, tb, tcc = ntmp(), ntmp(), ntmp()
                ka, kb, kcx = ("tmp%d" % ta, "*"), ("tmp%d" % tb, "*"), ("tmp%d" % tcc, "*")
                sc.op("dve", lambda e, ta=ta: e.tensor_copy(out=tmp[ta][:], in_=posi[:]), reads=[("posi", "*")], writes=[ka])
                sc.op("dve", lambda e, ta=ta: e.tensor_scalar(out=tmp[ta][:], in0=tmp[ta][:], scalar1=C("freq"),
                                                              scalar2=None, op0=ALU.mult),
                      reads=[ka, ("cp", "*")], writes=[ka])
                for which, dstt, dkey in ((0, ropeS, ("ropeS", "*")), (1, ropeC, ("ropeC", "*"))):
                    if which == 1:
                        sc.op("dve", lambda e, ta=ta: e.tensor_scalar(out=tmp[ta][:], in0=tmp[ta][:], scalar1=math.pi / 2,
                                                                      scalar2=None, op0=ALU.add), reads=[ka], writes=[ka])
                    sc.op("dve", lambda e, ta=ta: e.tensor_scalar(out=posi[:], in0=tmp[ta][:], scalar1=1.0 / (2 * math.pi),
                                                                  scalar2=None, op0=ALU.mult),
                          reads=[ka], writes=[("posi", "*")])
                    sc.op("dve", lambda e, tb=tb: e.tensor_copy(out=tmp[tb][:], in_=posi[:]), reads=[("posi", "*")],
                          writes=[kb])
                    sc.op("dve", lambda e, ta=ta, tb=tb: e.scalar_tensor_tensor(
                        out=tmp[tb][:], in0=tmp[tb][:], scalar=-2 * math.pi, in1=tmp[ta][:], op0=ALU.mult, op1=ALU.add),
                        reads=[ka, kb], writes=[kb])
                    sc.op("dve", lambda e, tb=tb, tcc=tcc: e.tensor_scalar(out=tmp[tcc][:], in0=tmp[tb][:], scalar1=math.pi,
                                                                          scalar2=2 * math.pi, op0=ALU.is_gt, op1=ALU.mult),
                          reads=[kb], writes=[kcx])
                    sc.op("dve", lambda e, tb=tb, tcc=tcc: e.tensor_tensor(out=tmp[tb][:], in0=tmp[tb][:], in1=tmp[tcc][:],
                                                                          op=ALU.subtract), reads=[kb, kcx], writes=[kb])
                    sc.op("act", lambda e, tb=tb, dstt=dstt: e.activation(out=dstt[:], in_=tmp[tb][:], func=AF.Sin),
                          reads=[kb], writes=[dkey])
                sc.op("dve", lambda e: e.tensor_scalar(out=ropeS[:], in0=ropeS[:], scalar1=C("sgn"), scalar2=None,
                                                       op0=ALU.mult), reads=[("ropeS", "*"), ("cp", "*")],
                      writes=[("ropeS", "*")])

            if it == 0:
                rope_tables(0)
            sc.op("dve", lambda e: e.tensor_copy(out=uT[:, :, 0:30], in_=uT[:, :, T:T + 30]), reads=[("uT", "*")],
                  writes=[("uT", "*")])
            rmsnorm("g_mix", T)
            for blk in range(8):
                s = wblock(wfm_d, blk * 256, 256)
                bA, bB = nbank(), nbank()
                proj(s, 0, T, bA)
                proj(s, 128, T, bB)
                t1, t2 = ntmp(), ntmp()
                k1, k2 = ("tmp%d" % t1, "*"), ("tmp%d" % t2, "*")
                sc.op("dve", lambda e, t1=t1, bA=bA: e.tensor_tensor(out=tmp[t1][:], in0=bank(bA), in1=ropeC[:],
                                                                    op=ALU.mult),
                      reads=[bkey(bA), ("ropeC", "*")], writes=[k1])
                sc.op("dve", lambda e, t2=t2, bB=bB: e.tensor_tensor(out=tmp[t2][:], in0=bank(bB), in1=ropeS[:],
                                                                    op=ALU.mult),
                      reads=[bkey(bB), ("ropeS", "*")], writes=[k2])
                if blk < 6:
                    dv, dk = BB[:, blk, :], ("BB", blk)
                elif blk == 6:
                    dv, dk = kT2[:, t0:t0 + T], ("kT2", it)
                else:
                    dv, dk = kiT2[:, t0:t0 + T], ("kiT2", it)
                sc.op("dve", lambda e, t1=t1, t2=t2, dv=dv: e.tensor_tensor(out=dv, in0=tmp[t1][:], in1=tmp[t2][:],
                                                                           op=ALU.add),
                      reads=[k1, k2], writes=[dk])
            for c in range(4):
                s = wblock(wfm_d, 2048 + c * 256, 256)
                bA, bB = nbank(), nbank()
                proj(s, 0, T, bA)
                proj(s, 128, T, bB)
                t1 = ntmp()
                k1 = ("tmp%d" % t1, "*")
                sc.op("act", lambda e, t1=t1, bB=bB: e.activation(out=tmp[t1][:], in_=bank(bB), func=AF.Tanh, scale=0.5),
                      reads=[bkey(bB)], writes=[k1])
                sc.op("dve", lambda e, t1=t1, bA=bA, c=c: e.scalar_tensor_tensor(
                    out=uT[:, c, 30:30 + T], in0=tmp[t1][:], scalar=1.0, in1=bank(bA), op0=ALU.add, op1=ALU.mult),
                    reads=[k1, bkey(bA)], writes=[("uT", "*")])
            for s4 in range(4):
                b = nbank()
                for kc in range(KC):
                    sc.op("pe", lambda e, b=b, kc=kc, s4=s4: e.matmul(bank(b)[:, 0:68], hT[:, kc, s4 * 128:(s4 + 1) * 128],
                                                                     wvw[:, kc, :], start=(kc == 0), stop=(kc == KC - 1)),
                          reads=[("wvw", "*"), ("hT", kc)], writes=[bkey(b)])
                qi_ = it * 4 + s4
                sc.op("act", lambda e, b=b, qi_=qi_: e.activation(out=vaug[:, qi_, 0:64], in_=bank(b)[:, 0:64],
                                                                  func=AF.Copy), reads=[bkey(b)], writes=[("vaug", qi_), ("vwtok", "*")])
                sc.op("dve", lambda e, b=b, s4=s4: e.tensor_scalar(out=wis[:, s4, :], in0=bank(b)[:, 64:68],
                                                                   scalar1=0.5 * 0.125, scalar2=None, op0=ALU.mult),
                      reads=[bkey(b), ("vwtok", "*")], writes=[("wis", s4)])

            if it + 1 < NT:
                x_load(it + 1, 0)
                x_load(it + 1, 1)

            def dsa_index(s4):
                j = it * 4 + s4
                n = (j + 1) * 128
                nck = (n + 511) // 512
                for ck in range(nck):
                    w = min(512, n - ck * 512)
                    for h in range(4):
                        b = nbank()
                        p0 = (h % 2) * 64
                        sc.op("pe", lambda e, b=b, h=h, p0=p0, ck=ck, w=w, s4=s4: e.matmul(
                            bank(b)[:, 0:w], BB[p0:p0 + 64, 4 + h // 2, s4 * 128:(s4 + 1) * 128],
                            kiT2[p0:p0 + 64, ck * 512:ck * 512 + w], start=True, stop=True),
                            reads=[("BB", 4 + h // 2), ("kiT2", "*")], writes=[bkey(b)])
                        r = rtmp[h % 2]
                        rk = ("rtmp%d" % (h % 2), "*")
                        sc.op("act", lambda e, b=b, r=r, w=w: e.activation(out=r[:, 0:w], in_=bank(b)[:, 0:w],
                                                                           func=AF.Relu), reads=[bkey(b)], writes=[rk])
                        sv = scores[:, ck * 512:ck * 512 + w]
                        if h == 0:
                            sc.op("dve", lambda e, r=r, w=w, sv=sv, s4=s4: e.tensor_scalar(
                                out=sv, in0=r[:, 0:w], scalar1=wis[:, s4, 0:1], scalar2=None, op0=ALU.mult),
                                reads=[rk, ("wis", s4)], writes=[("scores", "*")])
                        else:
                            sc.op("dve", lambda e, r=r, w=w, sv=sv, s4=s4, h=h: e.scalar_tensor_tensor(
                                out=sv, in0=r[:, 0:w], scalar=wis[:, s4, h:h + 1], in1=sv, op0=ALU.mult, op1=ALU.add),
                                reads=[rk, ("wis", s4), ("scores", "*")], writes=[("scores", "*")])
                sc.op("dve", lambda e, j=j: e.tensor_tensor(out=scores[:, j * 128:(j + 1) * 128],
                                                            in0=scores[:, j * 128:(j + 1) * 128], in1=causal,
                                                            op=ALU.add),
                      reads=[("scores", "*"), ("cmf", "*")], writes=[("scores", "*")])

            def dsa_bisect(s4):
                j = it * 4 + s4
                n = (j + 1) * 128
                nA = ((n * 7 // 20) // 128) * 128
                maskb = maskbs[j % 2]
                mk = ("maskb%d" % (j % 2), "*")
                junkA = prodT[:].rearrange("p a b -> p (a b)")
                sc.op("dve", lambda e: e.memset(bis[:, 2:3], 2.0 ** -13), writes=[("bis", 2)])
                for i in range(NBIS):
                    dl = 8.0 / (2 ** i)
                    nxt = dl / 2 if i < NBIS - 1 else dl
                    if nA > 0:
                        sc.op("act", lambda e, nA=nA: e.activation(
                            out=junkA[:, 0:nA], in_=scores[:, 0:nA], func=AF.Sign, bias=bis[:, 2:3], scale=-1.0,
                            accum_out=bis[:, 3:4]),
                            reads=[("scores", "*"), ("bis", 2)], writes=[("prodT", "*"), ("bis", 3)])
                    sc.op("dve", lambda e, n=n, nA=nA, maskb=maskb: e.tensor_scalar(
                        out=maskb[:, nA:n], in0=scores[:, nA:n], scalar1=bis[:, 2:3], scalar2=0.0, op0=ALU.is_gt,
                        op1=ALU.add, accum_out=bis[:, 0:1]),
                        reads=[("scores", "*"), ("bis", 2)], writes=[mk, ("bis", 0)])
                    if nA > 0:
                        sc.op("dve", lambda e: e.scalar_tensor_tensor(
                            out=bis[:, 1:2], in0=bis[:, 0:1], scalar=2.0, in1=bis[:, 3:4], op0=ALU.mult,
                            op1=ALU.subtract), reads=[("bis", 0), ("bis", 3)], writes=[("bis", 1)])
                        sc.op("dve", lambda e, dl=dl, nA=nA: e.tensor_scalar(
                            out=bis[:, 1:2], in0=bis[:, 1:2], scalar1=2.0 * TOPK - 1.0 - nA, scalar2=dl, op0=ALU.is_ge,
                            op1=ALU.mult), reads=[("bis", 1)], writes=[("bis", 1)])
                    else:
                        sc.op("dve", lambda e, dl=dl: e.tensor_scalar(
                            out=bis[:, 1:2], in0=bis[:, 0:1], scalar1=TOPK - 0.5, scalar2=dl, op0=ALU.is_ge,
                            op1=ALU.mult), reads=[("bis", 0)], writes=[("bis", 1)])
                    sc.op("dve", lambda e, nxt=nxt: e.scalar_tensor_tensor(out=bis[:, 2:3], in0=bis[:, 1:2], scalar=-nxt,
                                                                           in1=bis[:, 2:3], op0=ALU.add, op1=ALU.add),
                          reads=[("bis", 1), ("bis", 2)], writes=[("bis", 2)])
                    yield
                sc.op("dve", lambda e, n=n, maskb=maskb: e.tensor_scalar(
                    out=maskb[:, 0:n], in0=scores[:, 0:n], scalar1=bis[:, 2:3], scalar2=NEG, op0=ALU.is_le,
                    op1=ALU.mult), reads=[("scores", "*"), ("bis", 2)], writes=[mk])
                yield

            def dsa_attend(s4):
                j = it * 4 + s4
                maskb = maskbs[j % 2]
                mk = ("maskb%d" % (j % 2), "*")
                def att_L(c):
                    pr = npair()
                    for par in range(2):
                        p0 = par * 64
                        ov = ps[pr][:, par * 512:(par + 1) * 512]
                        sc.op("pe", lambda e, ov=ov, p0=p0, c=c, s4=s4: e.matmul(
                            ov.rearrange("p (a b) -> p a b", a=4), kT2[p0:p0 + 64, c * 128:(c + 1) * 128],
                            BB[p0:p0 + 64, 0:4, s4 * 128:(s4 + 1) * 128], start=True, stop=False),
                            reads=[("kT2", "*"), ("BB", 0), ("BB", 1), ("BB", 2), ("BB", 3)],
                            writes=[bkey(2 * pr + par)])
                    for par in range(2):
                        ov = ps[pr][:, par * 512:(par + 1) * 512]
                        sc.op("pe", lambda e, ov=ov, c=c, maskb=maskb: e.matmul(
                            ov, maskb[:, c * 128:(c + 1) * 128], E4, start=False, stop=True),
                            reads=[mk, ("cmb", "*")], writes=[bkey(2 * pr + par)])
                    pt = PT[c % 2]
                    pk = ("PT%d" % (c % 2), "*")
                    sc.op("act", lambda e, pr=pr, pt=pt: e.activation(out=pt[:], in_=ps[pr][:], func=AF.Exp, scale=0.125),
                          reads=[bkey(2 * pr), bkey(2 * pr + 1)], writes=[pk])

                def att_PV(c):
                    pt = PT[c % 2]
                    pk = ("PT%d" % (c % 2), "*")
                    for par in range(2):
                        sc.op("pe", lambda e, par=par, pt=pt, c=c, j=j: e.matmul(
                            ps[3][:, par * 512:(par + 1) * 512], vaug[:, c, :], pt[:, par * 512:(par + 1) * 512],
                            start=(c == 0), stop=(c == j)), reads=[pk, ("vaug", c)], writes=[bkey(6 + par)])

                att_L(0)
                yield
                for c in range(1, j + 1):
                    att_L(c)
                    att_PV(c - 1)
                    yield
                att_PV(j)
                sc.op("act", lambda e: e.activation(out=rec[64:128, :], in_=ps[3][64:128, :], func=AF.Ln),
                      reads=[bkey(6), bkey(7)], writes=[("rec", "*")])
                sc.op("act", lambda e: e.activation(out=rec[64:128, :], in_=rec[64:128, :], func=AF.Exp, scale=-1.0),
                      reads=[("rec", "*")], writes=[("rec", "*")])
                for par in range(2):
                    sc.op("dve", lambda e, par=par, s4=s4: e.tensor_tensor(
                        out=BC[par * 64:(par + 1) * 64, 0:4, s4 * 128:(s4 + 1) * 128],
                        in0=ps[3][0:64, par * 512:(par + 1) * 512].rearrange("p (a b) -> p a b", a=4),
                        in1=rec[64:128, par * 512:(par + 1) * 512].rearrange("p (a b) -> p a b", a=4), op=ALU.mult),
                        reads=[("rec", "*"), bkey(6 + par)], writes=[("BC", a_) for a_ in range(4)])

            def run_pair(ga, na, gb, nb):
                ia = ib = 0
                da = db = False
                while not (da and db):
                    take_b = (not db) and (da or ib * na <= ia * nb)
                    try:
                        next(gb if take_b else ga)
                    except StopIteration:
                        if take_b:
                            db = True
                        else:
                            da = True
                    else:
                        if take_b:
                            ib += 1
                        else:
                            ia += 1

            st["wide"] = False
            dsa_index(0)
            for _ in dsa_bisect(0):
                pass
            for s4 in range(1, 4):
                dsa_index(s4)
                run_pair(dsa_bisect(s4), NBIS + 1, dsa_attend(s4 - 1), it * 4 + s4 + 1)
            for _ in dsa_attend(3):
                pass
            st["wide"] = True

            yv = scores[:, 0:2048].rearrange("p (c t) -> p c t", c=4)
            bS, bQ = nbank(), nbank()
            for c in range(4):
                b = nbank()
                for jt in range(31):
                    pv = pcv[jt % 4]
                    pk = ("pcv%d" % (jt % 4), "*")
                    if jt % 3 == 0:
                        sc.op("act", lambda e, pv=pv, c=c, jt=jt: e.activation(
                            out=pv[:], in_=uT[:, c, jt:jt + T], func=AF.Copy, scale=cbw(c, jt)),
                            reads=[("uT", "*"), ("cder", "*")], writes=[pk])
                    else:
                        sc.op("dve", lambda e, pv=pv, c=c, jt=jt: e.tensor_scalar(
                            out=pv[:], in0=uT[:, c, jt:jt + T], scalar1=cbw(c, jt), scalar2=None, op0=ALU.mult),
                            reads=[("uT", "*"), ("cder", "*")], writes=[pk])
                    sc.op("pe", lambda e, b=b, pv=pv, jt=jt: e.matmul(bank(b), E4[:, 0:128], pv[:], start=(jt == 0),
                                                                     stop=(jt == 30)),
                          reads=[pk, ("cmb", "*")], writes=[bkey(b)])
                tb = ntmp()
                kb = ("tmp%d" % tb, "*")
                sc.op("act", lambda e, b=b, c=c: e.activation(out=yv[:, c, :], in_=bank(b), func=AF.Identity,
                                                              bias=C("cb_b", c)),
                      reads=[bkey(b), ("cp", "*")], writes=[("scores", "*")])
                sc.op("act", lambda e, tb=tb, c=c: e.activation(out=tmp[tb][:], in_=yv[:, c, :], func=AF.Square),
                      reads=[("scores", "*")], writes=[kb])
                sc.op("pe", lambda e, c=c, bS=bS: e.matmul(bank(bS), ones_f, yv[:, c, :], start=(c == 0), stop=(c == 3)),
                      reads=[("scores", "*"), ("cmf", "*")], writes=[bkey(bS)])
                sc.op("pe", lambda e, c=c, bQ=bQ, tb=tb: e.matmul(bank(bQ), ones_f, tmp[tb][:], start=(c == 0),
                                                                 stop=(c == 3)),
                      reads=[kb, ("cmf", "*")], writes=[bkey(bQ)])
            tm, tv, t2 = ntmp(), ntmp(), ntmp()
            km, kv, k2 = ("tmp%d" % tm, "*"), ("tmp%d" % tv, "*"), ("tmp%d" % t2, "*")
            sc.op("act", lambda e, tm=tm, bS=bS: e.activation(out=tmp[tm][:], in_=bank(bS), func=AF.Copy, scale=1.0 / 512),
                  reads=[bkey(bS)], writes=[km])
            sc.op("dve", lambda e, tm=tm, t2=t2: e.tensor_tensor(out=tmp[t2][:], in0=tmp[tm][:], in1=tmp[tm][:],
                                                                op=ALU.mult), reads=[km], writes=[k2])
            sc.op("dve", lambda e, tv=tv, bQ=bQ: e.tensor_scalar(out=tmp[tv][:], in0=bank(bQ), scalar1=1.0 / 512,
                                                                scalar2=EPS, op0=ALU.mult, op1=ALU.add),
                  reads=[bkey(bQ)], writes=[kv])
            sc.op("dve", lambda e, tv=tv, t2=t2: e.tensor_tensor(out=tmp[tv][:], in0=tmp[tv][:], in1=tmp[t2][:],
                                                                op=ALU.subtract), reads=[kv, k2], writes=[kv])
            sc.op("act", lambda e, tv=tv: e.activation(out=tmp[tv][:], in_=tmp[tv][:], func=AF.Ln),
                  reads=[kv], writes=[kv])
            sc.op("act", lambda e, tv=tv: e.activation(out=tmp[tv][:], in_=tmp[tv][:], func=AF.Exp, scale=-0.5),
                  reads=[kv], writes=[kv])
            for c in range(4):
                ta, tb = ntmp(), ntmp()
                while ta in (tm, tv) or tb in (tm, tv) or ta == tb:
                    ta, tb = ntmp(), ntmp()
                ka, kb = ("tmp%d" % ta, "*"), ("tmp%d" % tb, "*")
                sc.op("dve", lambda e, ta=ta, c=c, tm=tm: e.tensor_tensor(out=tmp[ta][:], in0=yv[:, c, :], in1=tmp[tm][:],
                                                                         op=ALU.subtract),
                      reads=[("scores", "*"), km], writes=[ka])
                sc.op("dve", lambda e, ta=ta, tv=tv: e.tensor_tensor(out=tmp[ta][:], in0=tmp[ta][:], in1=tmp[tv][:],
                                                                    op=ALU.mult), reads=[ka, kv], writes=[ka])
                sc.op("dve", lambda e, ta=ta, c=c: e.tensor_scalar(out=tmp[ta][:], in0=tmp[ta][:], scalar1=lng(c),
                                                                  scalar2=lnb(c), op0=ALU.mult, op1=ALU.add),
                      reads=[ka, ("cder", "*")], writes=[ka])
                sc.op("act", lambda e, ta=ta, tb=tb: e.activation(out=tmp[tb][:], in_=tmp[ta][:], func=AF.Tanh),
                      reads=[ka], writes=[kb])
                sc.op("dve", lambda e, ta=ta, tb=tb, c=c: e.scalar_tensor_tensor(
                    out=BC[:, 4 + c, :], in0=tmp[tb][:], scalar=1.0, in1=tmp[ta][:], op0=ALU.add, op1=ALU.mult),
                    reads=[ka, kb], writes=[("BC", 4 + c)])
            for dc in range(8):
                s = wblock(wout_d, dc * 128, 128)
                b = nbank()
                proj(s, 0, T, b, rhs_fn=lambda kc: BC[:, kc, :], rkeys=lambda kc: ("BC", kc), ncols=128)
                sc.op("dve", lambda e, b=b, dc=dc: e.tensor_tensor(out=xT[:, dc, :], in0=xT[:, dc, :], in1=bank(b),
                                                                  op=ALU.add),
                      reads=[bkey(b), ("xT", dc)], writes=[("xT", dc)])

            rmsnorm("g_cross", T)
            for blk in range(4):
                s = wblock(wq_d, blk * 256, 256)
                for cc in range(2):
                    b = nbank()
                    proj(s, cc * 128, T, b)
                    c = blk * 2 + cc
                    sc.op("act", lambda e, b=b, c=c: e.activation(out=BB[:, c, :], in_=bank(b), func=AF.Copy),
                          reads=[bkey(b)], writes=[("BB", c)])
            for h in range(4):
                pts = []
                for mc in range(2):
                    b = nbank()
                    for kk in range(2):
                        sc.op("pe", lambda e, b=b, h=h, kk=kk, mc=mc: e.matmul(
                            bank(b), kcT[:, 2 * h + kk, mc * 128:(mc + 1) * 128], BB[:, 2 * h + kk, :], start=(kk == 0),
                            stop=(kk == 1)), reads=[("kcT", 2 * h + kk), ("BB", 2 * h + kk)], writes=[bkey(b)])
                    pt = PT[mc][:, 0:512]
                    pk = ("PT%d" % mc, "*")
                    sc.op("act", lambda e, b=b, pt=pt: e.activation(out=pt, in_=bank(b), func=AF.Exp, scale=1.0 / 16),
                          reads=[bkey(b)], writes=[pk])
                    pts.append((pt, pk))
                bD = nbank()
                for mc in range(2):
                    sc.op("pe", lambda e, bD=bD, mc=mc, pt=pts[mc][0]: e.matmul(bank(bD), ones_b, pt, start=(mc == 0),
                                                                              stop=(mc == 1)),
                          reads=[pts[mc][1], ("cmb", "*")], writes=[bkey(bD)])
                tr = ntmp()
                kr = ("tmp%d" % tr, "*")
                sc.op("act", lambda e, tr=tr, bD=bD: e.activation(out=tmp[tr][:], in_=bank(bD), func=AF.Ln),
                      reads=[bkey(bD)], writes=[kr])
                sc.op("act", lambda e, tr=tr: e.activation(out=tmp[tr][:], in_=tmp[tr][:], func=AF.Exp, scale=-1.0),
                      reads=[kr], writes=[kr])
                for dd in range(2):
                    b = nbank()
                    cdx = 2 * h + dd
                    for mc in range(2):
                        sc.op("pe", lambda e, b=b, mc=mc, cdx=cdx, pt=pts[mc][0]: e.matmul(
                            bank(b), vc[:, mc, cdx * 128:(cdx + 1) * 128], pt, start=(mc == 0), stop=(mc == 1)),
                            reads=[pts[mc][1], ("vc", "*")], writes=[bkey(b)])
                    sc.op("dve", lambda e, b=b, cdx=cdx, tr=tr: e.tensor_tensor(out=BC[:, cdx, :], in0=bank(b),
                                                                                in1=tmp[tr][:], op=ALU.mult),
                          reads=[bkey(b), kr], writes=[("BC", cdx)])
            for blk in range(4):
                s = wblock(wo_d, blk * 256, 256)
                for cc in range(2):
                    b = nbank()
                    dc = blk * 2 + cc
                    proj(s, cc * 128, T, b, rhs_fn=lambda kc: BC[:, kc, :], rkeys=lambda kc: ("BC", kc))
                    sc.op("dve", lambda e, b=b, dc=dc: e.tensor_tensor(out=xT[:, dc, :], in0=xT[:, dc, :], in1=bank(b),
                                                                      op=ALU.add),
                          reads=[bkey(b), ("xT", dc)], writes=[("xT", dc)])

            rmsnorm("g_ffn", T)
            if it + 1 < NT:
                rope_tables(it + 1)
            for g in range(NF // FG):
                def ffn_A1(fl):
                    f = g * FG + fl
                    srcG = wg_d[:, f * 128:(f + 1) * 128].rearrange("(kc p) n -> p kc n", p=128)
                    srcU = wu_d[:, f * 128:(f + 1) * 128].rearrange("(kc p) n -> p kc n", p=128)
                    s = wload([(lambda r: r[:, 0:1024].rearrange("p (kc n) -> p kc n", kc=8), srcG),
                               (lambda r: r[:, 1024:2048].rearrange("p (kc n) -> p kc n", kc=8), srcU)])
                    bG, bU = nbank(), nbank()
                    proj(s, 0, T, bG, ncols=128, off=0)
                    proj(s, 0, T, bU, ncols=128, off=1024)
                    gs = Gs[f % 2]
                    gk = ("Gs%d" % (f % 2), "*")
                    sc.op("dve", lambda e, gs=gs, f=f: e.tensor_copy(out=gs[:, 0:2], in_=halo[:, f, :]),
                          reads=[("halo", f)], writes=[gk])
                    sc.op("act", lambda e, gs=gs, bG=bG: e.activation(out=gs[:, 2:2 + T], in_=bank(bG), func=AF.Copy),
                          reads=[bkey(bG)], writes=[gk])
                    ta, tb = ntmp(), ntmp()
                    ka, kb = ("tmp%d" % ta, "*"), ("tmp%d" % tb, "*")
                    fw = lambda jj, f=f: cp[:, coff["f_w"] + f * 3 + jj:coff["f_w"] + f * 3 + jj + 1]
                    sc.op("act", lambda e, ta=ta, bG=bG, f=f, fw=fw: e.activation(
                        out=tmp[ta][:], in_=bank(bG), func=AF.Identity, scale=fw(2), bias=C("f_b", f)),
                        reads=[bkey(bG), ("cp", "*")], writes=[ka])
                    sc.op("act", lambda e, gs=gs, f=f: e.activation(out=halo[:, f, :], in_=gs[:, T:T + 2], func=AF.Copy),
                          reads=[gk], writes=[("halo", f)])
                    return (ta, tb, ka, kb, bU, gs, gk, fw)

                def ffn_A2(stt_):
                    ta, tb, ka, kb, bU, gs, gk, fw = stt_
                    for jj in (1, 0):
                        sc.op("dve", lambda e, ta=ta, gs=gs, jj=jj, fw=fw: e.scalar_tensor_tensor(
                            out=tmp[ta][:], in0=gs[:, jj:jj + T], scalar=fw(jj), in1=tmp[ta][:], op0=ALU.mult,
                            op1=ALU.add), reads=[gk, ("cp", "*"), ka], writes=[ka])
                    sc.op("act", lambda e, ta=ta, tb=tb: e.activation(out=tmp[tb][:], in_=tmp[ta][:], func=AF.Tanh,
                                                                      scale=0.5), reads=[ka], writes=[kb])

                def ffn_B(fl, stt_):
                    ta, tb, ka, kb, bU = stt_[:5]
                    sc.op("dve", lambda e, ta=ta, tb=tb: e.scalar_tensor_tensor(
                        out=tmp[tb][:], in0=tmp[tb][:], scalar=1.0, in1=tmp[ta][:], op0=ALU.add, op1=ALU.mult),
                        reads=[ka, kb], writes=[kb])
                    sc.op("dve", lambda e, tb=tb, bU=bU, fl=fl: e.tensor_tensor(out=prodT[:, fl, :], in0=tmp[tb][:],
                                                                               in1=bank(bU), op=ALU.mult),
                          reads=[kb, bkey(bU)], writes=[("prodT", fl)])

                sts_ = {}
                for step in range(FG + 2):
                    if step < FG:
                        sts_[step] = ffn_A1(step)
                    if 0 <= step - 1 < FG:
                        ffn_A2(sts_[step - 1])
                    if 0 <= step - 2 < FG:
                        ffn_B(step - 2, sts_[step - 2])
                for dc in range(8):
                    src = wd_d[g * FG * 128:(g + 1) * FG * 128, dc * 128:(dc + 1) * 128].rearrange(
                        "(kc p) n -> p kc n", p=128)
                    s = wload([(lambda r: r[:, 0:FG * 128].rearrange("p (kc n) -> p kc n", kc=FG), src)])
                    b = nbank()
                    proj(s, 0, T, b, rhs_fn=lambda kc: prodT[:, kc, :], rkeys=lambda kc: ("prodT", kc), nk=FG, ncols=128)
                    sc.op("dve", lambda e, b=b, dc=dc: e.scalar_tensor_tensor(
                        out=xT[:, dc, :], in0=bank(b), scalar=0.5, in1=xT[:, dc, :], op0=ALU.mult, op1=ALU.add),
                        reads=[bkey(b), ("xT", dc)], writes=[("xT", dc)])

            rmsnorm("g_fin", T, inplace=True)
            for s4 in range(4):
                for half in range(2):
                    b = nbank()
                    for i in range(4):
                        dc = half * 4 + i
                        sc.op("pe", lambda e, b=b, i=i, dc=dc, s4=s4: e.transpose(
                            out=bank(b)[:, i * 128:(i + 1) * 128], in_=xT[:, dc, s4 * 128:(s4 + 1) * 128],
                            identity=ident), reads=[("xT", dc), ("cmf", "*")], writes=[bkey(b)])
                    if half == 0:
                        sc.op("act", lambda e, b=b, s4=s4: e.activation(out=osts[s4 % 2][:, 0:512], in_=bank(b),
                                                                        func=AF.Copy),
                              reads=[bkey(b)], writes=[("ost%d" % (s4 % 2), 0)])
                    else:
                        sc.op("dve", lambda e, b=b, s4=s4: e.tensor_copy(out=osts[s4 % 2][:, 512:1024], in_=bank(b)),
                              reads=[bkey(b)], writes=[("ost%d" % (s4 % 2), 1)])
                sc.dma("sp", lambda e, r0=t0 + s4 * 128, s4=s4: e.dma_start(out=out_d[r0:r0 + 128, :],
                                                                            in_=osts[s4 % 2][:]),
                       "ost%d" % (s4 % 2), reads=[("ost%d" % (s4 % 2), "*")])
        sc.op("sp", None, writes=[("ost0", "*"), ("ost1", "*")])
        block = es.enter_context(nc.Block())
        sc.emit(block)
    return nc


def _swap_cols(n_heads):
    idx = []
    for h in range(n_heads):
        base = h * 64
        idx += [base + 8 + i for i in range(8)] + [base + i for i in range(8)] + [base + i for i in range(16, 64)]
    return np.array(idx)


def host_layout(inputs):
    f = lambda k: np.asarray(inputs[k], dtype=np.float32)
    w_in = f("w_in")[0]
    q, k, v, qi, ki, wi, glu = np.split(w_in, np.cumsum([512, 64, 64, 256, 64, 4, 1024])[:-1], axis=1)
    base = np.concatenate([q, qi, k, k, ki, ki], axis=1)
    sw = base[:, _swap_cols(16)]
    ga, gg = glu[:, :512], glu[:, 512:]
    blocks = []
    for c in range(8):
        blocks += [base[:, c * 128:(c + 1) * 128], sw[:, c * 128:(c + 1) * 128]]
    for c in range(4):
        blocks += [ga[:, c * 128:(c + 1) * 128], gg[:, c * 128:(c + 1) * 128]]
    w_fm = np.ascontiguousarray(np.concatenate(blocks, axis=1))
    w_vw = np.ascontiguousarray(np.concatenate([v, wi], axis=1))
    coff, ncp = cpack_layout()
    cpk = np.zeros((128, ncp), np.float32)
    col = lambda vec, n: vec.reshape(n, 128).T
    cpk[:, coff["g_mix"]:coff["g_mix"] + 8] = col(f("norm_mix_g")[0], 8)
    cpk[:, coff["g_cross"]:coff["g_cross"] + 8] = col(f("norm_cross_g")[0], 8)
    cpk[:, coff["g_mem"]:coff["g_mem"] + 8] = col(f("norm_mem_g")[0], 8)
    cpk[:, coff["g_ffn"]:coff["g_ffn"] + 8] = col(f("norm_ffn_g")[0], 8)
    cpk[:, coff["g_fin"]:coff["g_fin"] + 8] = col(f("norm_final_g"), 8)
    cpk[:, coff["ln_g"]:coff["ln_g"] + 4] = col(f("ln_b_g")[0], 4)
    cpk[:, coff["ln_b"]:coff["ln_b"] + 4] = col(f("ln_b_b")[0], 4)
    cpk[:, coff["cb_b"]:coff["cb_b"] + 4] = col(f("conv_b_b")[0], 4)
    cw = f("conv_b_w")[0]
    cpk[:, coff["cb_w"]:coff["cb_w"] + 124] = cw.reshape(31, 4, 128).transpose(2, 1, 0).reshape(128, 124)
    fw = f("ffn_conv_w")[0]
    cpk[:, coff["f_w"]:coff["f_w"] + NF * 3] = fw.reshape(3, NF, 128).transpose(2, 1, 0).reshape(128, NF * 3)
    cpk[:, coff["f_b"]:coff["f_b"] + NF] = col(f("ffn_conv_b")[0], NF)
    p = np.arange(128) % 64
    freqs = (np.float32(500000.0) ** (-np.arange(0, 16, 2, dtype=np.float32) / np.float32(16))).astype(np.float32)
    cpk[:, coff["freq"]] = np.where(p < 16, freqs[p % 8], 0.0)
    cpk[:, coff["sgn"]] = np.where(p < 8, -1.0, np.where(p < 16, 1.0, 0.0))
    cm = np.zeros((128, 896), np.float32)
    cm[:, 0:128] = np.eye(128)
    qq, kk = np.meshgrid(np.arange(128), np.arange(128), indexing="ij")
    cm[:, 128:256] = np.where(kk <= qq, 0.0, -1e30)
    cm[:, 256:384] = 1.0
    cm[:, 384:896] = np.tile(np.eye(128, dtype=np.float32), (1, 4))
    shared = {
        "w_fm": w_fm, "w_vw": w_vw, "w_out": f("w_out")[0], "w_q": f("w_q_cross")[0], "w_k": f("w_k_cross")[0],
        "w_v": f("w_v_cross")[0], "w_o": f("w_o_cross")[0], "w_gate": f("w_gate")[0], "w_up": f("w_up")[0],
        "w_down": f("w_down")[0], "cpack": cpk, "cmats": cm,
    }
    return shared


_NC_CACHE = {}


def kernel(**inputs):
    x = np.asarray(inputs["x"], dtype=np.float32)
    mem = np.asarray(inputs["mem"], dtype=np.float32)
    pos = np.asarray(inputs["positions"], dtype=np.int32)
    B, S, _ = x.shape
    shared = host_layout(inputs)
    if S not in _NC_CACHE:
        _NC_CACHE[S] = build(S)
    nc = _NC_CACHE[S]
    in_maps = []
    for b in range(B):
        m = dict(shared)
        m["x"] = np.ascontiguousarray(x[b])
        m["mem"] = np.ascontiguousarray(mem[b])
        m["pos"] = np.ascontiguousarray(pos[b][None, :])
        in_maps.append(m)
    res = run_bass_kernel_spmd(nc, in_maps, core_ids=list(range(B)))
    return np.stack([np.asarray(r["out"], dtype=np.float32) for r in res.results], axis=0)
```

```python
import math
import contextlib
import numpy as np
import concourse.bass as bass
import concourse.mybir as mybir
from concourse.bass_utils import run_bass_kernel_spmd

F32 = mybir.dt.float32
BF16 = mybir.dt.bfloat16
I32 = mybir.dt.int32
ALU = mybir.AluOpType
AF = mybir.ActivationFunctionType

D = 1024
KC = 8
T = 512
NMEM = 256
DFF = 2816
NF = 22
FG = 11
EPS = 1e-6
TOPK = 256
NEG = -30000.0
NBIS = 14
RING = 5
SLOT = 2048


class Sched:
    ENGS = ("pe", "act", "dve", "pool", "sp")

    def __init__(self, nc, es):
        self.nc = nc
        self.ops = {e: [] for e in self.ENGS}
        self.res = {}
        self.esem = {e: es.enter_context(nc.semaphore("se_" + e)) for e in self.ENGS}
        self.dsem = {}
        self.es = es

    def _new(self):
        return {"w": None, "r": {}}

    def _states(self, buf, key):
        d = self.res.setdefault(buf, {})
        if "*" not in d:
            d["*"] = self._new()
        if key == "*":
            return list(d.values())
        if key not in d:
            d[key] = self._new()
            d[key]["w"] = d["*"]["w"]
            d[key]["r"] = dict(d["*"]["r"])
        return [d[key], d["*"]]

    def _gather(self, eng, reads, writes):
        raw, other = set(), set()
        for (buf, key) in reads:
            for st in self._states(buf, key):
                if st["w"] is not None:
                    raw.add(st["w"])
        for (buf, key) in writes:
            for st in self._states(buf, key):
                if st["w"] is not None:
                    other.add(st["w"])
                other.update(st["r"].values())
        deps = set()
        for dpn in raw:
            if dpn[0] == "E" and dpn[1] == eng and eng in ("pe", "sp"):
                continue
            deps.add(dpn)
        for dpn in other:
            if dpn[0] == "E" and dpn[1] == eng and eng in ("pe", "sp"):
                continue
            deps.add(dpn)
        return deps

    def _record(self, me, rkey, reads, writes):
        for (buf, key) in reads:
            sts = self._states(buf, key)
            if key != "*":
                sts = sts[:1]
            for st in sts:
                st["r"][rkey] = me
        for (buf, key) in writes:
            d = self.res[buf]
            if key == "*":
                for st in d.values():
                    st["w"] = me
                    st["r"] = {}
            else:
                d[key]["w"] = me
                d[key]["r"] = {}

    def op(self, eng, fn, reads=(), writes=()):
        idx = len(self.ops[eng])
        me = ("E", eng, idx)
        deps = self._gather(eng, reads, writes)
        self._record(me, ("E", eng), reads, writes)
        self.ops[eng].append((fn, deps, None))

    def dma(self, eng, fn, sem, reads=(), writes=()):
        if sem not in self.dsem:
            self.dsem[sem] = [self.es.enter_context(self.nc.semaphore("sd_" + sem)), 0]
        self.dsem[sem][1] += 16
        me = ("S", sem, self.dsem[sem][1])
        deps = {dp for dp in self._gather(eng, reads, writes) if not (dp[0] == "S" and dp[1] == sem)}
        self._record(me, ("S", sem), reads, writes)
        self.ops[eng].append((fn, deps, sem))

    def emit(self, block):
        signal = {e: set() for e in self.ENGS}
        for e in self.ENGS:
            for (_, deps, _) in self.ops[e]:
                for dpn in deps:
                    if dpn[0] == "E":
                        signal[dpn[1]].add(dpn[2])
        rank = {}
        for e in self.ENGS:
            r, c = {}, 0
            for i in sorted(signal[e]):
                c += 1
                r[i] = c
            rank[e] = r
        self.nsignal = {e: len(rank[e]) for e in self.ENGS}

        def run(ename, eng):
            known = {}
            for i, (fn, deps, dsem) in enumerate(self.ops[ename]):
                waits = {}
                for dpn in deps:
                    if dpn[0] == "E":
                        k, h, v = ("E", dpn[1]), self.esem[dpn[1]], rank[dpn[1]][dpn[2]]
                    else:
                        k, h, v = ("S", dpn[1]), self.dsem[dpn[1]][0], dpn[2]
                    if v > known.get(k, 0) and v > waits.get(k, (None, 0))[1]:
                        waits[k] = (h, v)
                for k, (h, v) in waits.items():
                    eng.wait_ge(h, v)
                    known[k] = v
                if fn is None:
                    continue
                ins = fn(eng)
                if dsem is not None:
                    ins.then_inc(self.dsem[dsem][0], 16)
                elif i in rank[ename]:
                    ins.then_inc(self.esem[ename], 1)

        block.tensor(lambda e: run("pe", e))
        block.scalar(lambda e: run("act", e))
        block.vector(lambda e: run("dve", e))
        block.gpsimd(lambda e: run("pool", e))
        block.sync(lambda e: run("sp", e))


def cpack_layout():
    off, cur = {}, 0
    for name, n in (("g_mix", 8), ("g_cross", 8), ("g_mem", 8), ("g_ffn", 8), ("g_fin", 8), ("ln_g", 4), ("ln_b", 4),
                    ("cb_b", 4), ("cb_w", 4 * 31), ("f_w", NF * 3), ("f_b", NF), ("freq", 1), ("sgn", 1)):
        off[name] = cur
        cur += n
    return off, cur


def build(S, debug=False):
    NT = S // T
    NQ = S // 128
    nc = bass.Bass("TRN2", target_bir_lowering=False)
    dram = lambda name, shape, dt=F32, kind="ExternalInput": nc.dram_tensor(name, shape, dt, kind=kind).ap()
    x_d = dram("x", [S, D])
    mem_d = dram("mem", [NMEM, D])
    pos_d = dram("pos", [1, S], I32)
    wfm_d = dram("w_fm", [D, 3072])
    wvw_d = dram("w_vw", [D, 68])
    wout_d = dram("w_out", [D, D])
    wq_d = dram("w_q", [D, D])
    wk_d = dram("w_k", [D, D])
    wv_d = dram("w_v", [D, D])
    wo_d = dram("w_o", [D, D])
    wg_d = dram("w_gate", [D, DFF])
    wu_d = dram("w_up", [D, DFF])
    wd_d = dram("w_down", [DFF, D])
    coff, ncp = cpack_layout()
    cp_d = dram("cpack", [128, ncp])
    cm_d = dram("cmats", [128, 128 * 3 + 512])
    out_d = dram("out", [S, D], kind="ExternalOutput")

    es = contextlib.ExitStack()
    with es:
        sb = lambda name, shape, dt=F32: es.enter_context(nc.sbuf_tensor(name, shape, dt))
        sc = Sched(nc, es)
        cp = sb("cp", [128, ncp])
        cmf = sb("cmf", [128, 384])
        cmb = sb("cmb", [128, 128 + 512], BF16)
        ident = cmf[:, 0:128]
        causal = cmf[:, 128:256]
        ones_f = cmf[:, 256:384]
        ones_b = cmb[:, 0:128]
        E4 = cmb[:, 128:640]
        cder = sb("cder", [128, 16 + 124])
        kT2 = sb("kT2", [128, S], BF16)
        kiT2 = sb("kiT2", [128, S], BF16)
        vaug = sb("vaug", [128, NQ, 128], BF16)
        kcT = sb("kcT", [128, 8, NMEM], BF16)
        vc = sb("vc", [128, 2, D], BF16)
        halo = sb("halo", [128, NF, 2])
        uT = sb("uT", [128, 4, 30 + T])
        xT = sb("xT", [128, KC, T])
        hT = sb("hT", [128, KC, T], BF16)
        BB = sb("BB", [128, 8, T], BF16)
        BC = sb("BC", [128, 8, T], BF16)
        prodT = sb("prodT", [128, FG, T], BF16)
        ropeC = sb("ropeC", [128, T])
        ropeS = sb("ropeS", [128, T])
        posi = sb("posi", [128, T], I32)
        scores = sb("scores", [128, max(S, 2048)])
        maskbs = [sb("maskb%d" % i, [128, S], BF16) for i in range(2)]
        pcv = [sb("pcv%d" % i, [128, T], BF16) for i in range(4)]
        rtmp = [sb("rtmp%d" % i, [128, T]) for i in range(2)]
        PT = [sb("PT%d" % i, [128, 1024], BF16) for i in range(2)]
        rec = sb("rec", [128, 1024])
        tmp = [sb("tmp%d" % i, [128, T]) for i in range(6)]
        sqt = [sb("sqt%d" % i, [128, T], BF16) for i in range(2)]
        Gs = [sb("Gs%d" % i, [128, T + 2]) for i in range(2)]
        xins = [sb("xin%d" % i, [128, D]) for i in range(2)]
        osts = [sb("ost%d" % i, [128, D]) for i in range(2)]
        wis = sb("wis", [128, 4, 4])
        bis = sb("bis", [128, 4])
        ring = [sb("ring%d" % i, [128, SLOT], BF16) for i in range(RING)]
        wvw = sb("wvw", [128, KC, 68], BF16)
        ps = [es.enter_context(nc.psum_tensor("ps%d" % i, [128, 1024], F32)) for i in range(4)]
        bank = lambda b: ps[b // 2][:, (b % 2) * 512:(b % 2 + 1) * 512]
        bkey = lambda b: ("ps", b)
        st = {"rr": 0, "pair": 0, "ring": 0, "t": 0, "blk": None, "it": 0, "wide": True}
        NT_ = NT
        NBLK = 12 + 8 + 8 + NF + 16
        wscr = nc.dram_tensor("wscr", [NBLK, 128, SLOT], BF16, kind="Internal").ap()

        def nbank():
            nb_ = 8 if st["wide"] else 6
            b = st["rr"] % nb_
            st["rr"] = (b + 1) % nb_
            return b

        def npair():
            p = st["pair"]
            st["pair"] = (p + 1) % 3
            return p

        def ntmp():
            t = st["t"]
            st["t"] = (t + 1) % 6
            return t

        C = lambda name, i=0: cp[:, coff[name] + i:coff[name] + i + 1]

        sc.dma("sp", lambda e: e.dma_start(out=cp[:], in_=cp_d[:, :]), "c0", writes=[("cp", "*")])
        sc.dma("sp", lambda e: e.dma_start(out=cmf[:], in_=cm_d[:, 0:384]), "c1", writes=[("cmf", "*")])
        sc.dma("pool", lambda e: e.dma_start(out=cmb[:], in_=cm_d[:, 256:896]), "c2", writes=[("cmb", "*")])
        sc.dma("pool", lambda e: e.dma_start(out=wvw[:], in_=wvw_d.rearrange("(kc p) n -> p kc n", p=128)), "c3",
               writes=[("wvw", "*")])
        sc.op("dve", lambda e: e.memset(halo[:], 0.0), writes=[("halo", "*")])
        sc.op("dve", lambda e: e.memset(uT[:], 0.0), writes=[("uT", "*")])
        sc.op("dve", lambda e: e.memset(vaug[:, :, 64:128], 1.0), writes=[("vaug", "*")])
        sc.op("dve", lambda e: e.tensor_scalar(out=cder[:, 0:8], in0=cp[:, coff["ln_g"]:coff["ln_g"] + 8], scalar1=0.5,
                                               scalar2=None, op0=ALU.mult), reads=[("cp", "*")], writes=[("cder", "*")])
        sc.op("dve", lambda e: e.tensor_scalar(out=cder[:, 16:140], in0=cp[:, coff["cb_w"]:coff["cb_w"] + 124],
                                               scalar1=0.5, scalar2=None, op0=ALU.mult),
              reads=[("cp", "*")], writes=[("cder", "*")])
        lng = lambda c: cder[:, c:c + 1]
        lnb = lambda c: cder[:, 4 + c:5 + c]
        cbw = lambda c, j: cder[:, 16 + c * 31 + j:16 + c * 31 + j + 1]

        def wload(dmas):
            s = st["ring"]
            st["ring"] = (s + 1) % RING
            bi = st["blk"]
            if bi is not None:
                st["blk"] = bi + 1
            if bi is None or st["it"] == 0:
                for (vf, src) in dmas:
                    sc.dma("pool", lambda e, vf=vf, src=src, s=s: e.dma_start(out=vf(ring[s]), in_=src), "ring%d" % s,
                           writes=[("ring%d" % s, "*")])
                if bi is not None and NT_ > 1:
                    sc.dma("sp", lambda e, s=s, bi=bi: e.dma_start(out=wscr[bi], in_=ring[s][:, :]), "rst%d" % s,
                           reads=[("ring%d" % s, "*")], writes=[("wscr", bi)])
            else:
                q = "sp" if bi % 2 == 0 else "pool"
                sc.dma(q, lambda e, s=s, bi=bi: e.dma_start(out=ring[s][:, :], in_=wscr[bi]),
                       ("rgs%d" if q == "sp" else "ring%d") % s, reads=[("wscr", bi)], writes=[("ring%d" % s, "*")])
            return s

        def wblock(wd, c0, ncols, k0=0, nk=KC):
            src = wd[k0 * 128:(k0 + nk) * 128, c0:c0 + ncols].rearrange("(kc p) n -> p kc n", p=128)
            return wload([(lambda r: r[:, 0:nk * ncols].rearrange("p (kc n) -> p kc n", kc=nk), src)])

        def rview(s, nk, ncols, off=0, parts=128):
            return ring[s][0:parts, off:off + nk * ncols].rearrange("p (kc n) -> p kc n", kc=nk)

        def transpose_in(src_tile, src_key, dst, dst_key, col0, ncol=128):
            for half in range(2):
                b = nbank()
                for i in range(4):
                    dc = half * 4 + i
                    sc.op("pe", lambda e, b=b, i=i, dc=dc: e.transpose(out=bank(b)[:, i * 128:(i + 1) * 128],
                                                                    in_=src_tile[:, dc * 128:(dc + 1) * 128],
                                                                    identity=ident),
                          reads=[src_key, ("cmf", "*")], writes=[bkey(b)])
                eng = "act" if half == 0 else "dve"
                dv = dst[:, half * 4:half * 4 + 4, col0:col0 + 128]
                sv = bank(b).rearrange("p (a b) -> p a b", a=4)
                if eng == "act":
                    sc.op("act", lambda e, dv=dv, sv=sv: e.activation(out=dv, in_=sv, func=AF.Copy),
                          reads=[bkey(b)], writes=[dst_key])
                else:
                    sc.op("dve", lambda e, dv=dv, sv=sv: e.tensor_copy(out=dv, in_=sv),
                          reads=[bkey(b)], writes=[dst_key])

        def rmsnorm(gname, n, dst=None, inplace=False):
            b = nbank()
            for kc in range(KC):
                q = sqt[kc % 2]
                qk = ("sqt%d" % (kc % 2), "*")
                sc.op("act", lambda e, q=q, kc=kc: e.activation(out=q[:, 0:n], in_=xT[:, kc, 0:n], func=AF.Square),
                      reads=[("xT", kc)], writes=[qk])
                sc.op("pe", lambda e, q=q, kc=kc, b=b: e.matmul(bank(b)[:, 0:n], ones_b, q[:, 0:n], start=(kc == 0),
                                                                 stop=(kc == KC - 1)),
                      reads=[qk, ("cmb", "*")], writes=[bkey(b)])
            t = ntmp()
            tk = ("tmp%d" % t, "*")
            sc.op("dve", lambda e, t=t, b=b: e.tensor_scalar(out=tmp[t][:, 0:n], in0=bank(b)[:, 0:n], scalar1=1.0 / D,
                                                            scalar2=EPS, op0=ALU.mult, op1=ALU.add),
                  reads=[bkey(b)], writes=[tk])
            sc.op("act", lambda e, t=t: e.activation(out=tmp[t][:, 0:n], in_=tmp[t][:, 0:n], func=AF.Ln),
                  reads=[tk], writes=[tk])
            sc.op("act", lambda e, t=t: e.activation(out=tmp[t][:, 0:n], in_=tmp[t][:, 0:n], func=AF.Exp, scale=-0.5),
                  reads=[tk], writes=[tk])
            for kc in range(KC):
                if inplace:
                    sc.op("dve", lambda e, t=t, kc=kc: e.scalar_tensor_tensor(
                        out=xT[:, kc, 0:n], in0=xT[:, kc, 0:n], scalar=C(gname, kc), in1=tmp[t][:, 0:n],
                        op0=ALU.mult, op1=ALU.mult), reads=[("xT", kc), tk, ("cp", "*")], writes=[("xT", kc)])
                else:
                    sc.op("dve", lambda e, t=t, kc=kc: e.scalar_tensor_tensor(
                        out=hT[:, kc, 0:n], in0=xT[:, kc, 0:n], scalar=C(gname, kc), in1=tmp[t][:, 0:n],
                        op0=ALU.mult, op1=ALU.mult), reads=[("xT", kc), tk, ("cp", "*")], writes=[("hT", kc)])

        def proj(s, col, n, b, rhs_fn=None, rkeys=None, nk=KC, ncols=256, off=0):
            wv = rview(s, nk, ncols, off)
            for kc in range(nk):
                rhs = hT[:, kc, 0:n] if rhs_fn is None else rhs_fn(kc)
                rk = ("hT", kc) if rkeys is None else rkeys(kc)
                sc.op("pe", lambda e, wv=wv, kc=kc, rhs=rhs: e.matmul(bank(b)[:, 0:n], wv[:, kc, col:col + 128], rhs,
                                                                      start=(kc == 0), stop=(kc == nk - 1)),
                      reads=[("ring%d" % s, "*"), rk], writes=[bkey(b)])

        for mc in range(2):
            sc.dma("sp", lambda e, mc=mc: e.dma_start(out=xins[mc][:], in_=mem_d[mc * 128:(mc + 1) * 128, :]),
                   "xin%d" % mc, writes=[("xin%d" % mc, "*")])
            transpose_in(xins[mc], ("xin%d" % mc, "*"), xT, ("xT", "*"), mc * 128)
        rmsnorm("g_mem", NMEM)
        for blk in range(4):
            s = wblock(wk_d, blk * 256, 256)
            for cc in range(2):
                b = nbank()
                proj(s, cc * 128, NMEM, b)
                c = blk * 2 + cc
                sc.op("act", lambda e, b=b, c=c: e.activation(out=kcT[:, c, :], in_=bank(b)[:, 0:NMEM], func=AF.Copy),
                      reads=[bkey(b)], writes=[("kcT", c)])
        for blk in range(4):
            s = wblock(wv_d, blk * 256, 256)
            wv_ = rview(s, KC, 256)
            for mc in range(2):
                b = nbank()
                for kc in range(KC):
                    sc.op("pe", lambda e, b=b, kc=kc, mc=mc, wv_=wv_: e.matmul(
                        bank(b)[:, 0:256], hT[:, kc, mc * 128:(mc + 1) * 128], wv_[:, kc, :], start=(kc == 0),
                        stop=(kc == KC - 1)), reads=[("ring%d" % s, "*"), ("hT", kc)], writes=[bkey(b)])
                sc.op("act", lambda e, b=b, mc=mc, blk=blk: e.activation(out=vc[:, mc, blk * 256:(blk + 1) * 256],
                                                                          in_=bank(b)[:, 0:256], func=AF.Copy),
                      reads=[bkey(b)], writes=[("vc", "*")])

        for it in range(NT):
            t0 = it * T
            st["blk"] = 0
            st["it"] = it
            def x_load(itx, s4):
                r0 = itx * T + s4 * 128
                sc.dma("sp", lambda e, r0=r0, s4=s4: e.dma_start(out=xins[s4 % 2][:], in_=x_d[r0:r0 + 128, :]),
                       "xin%d" % (s4 % 2), writes=[("xin%d" % (s4 % 2), "*")])

            for s4 in range(4):
                if it == 0 or s4 >= 2:
                    x_load(it, s4)
                transpose_in(xins[s4 % 2], ("xin%d" % (s4 % 2), "*"), xT, ("xT", "*"), s4 * 128)
            def rope_tables(itx):
                t0r = itx * T
                sc.dma("sp", lambda e, t0=t0r: e.dma_start(out=posi[:], in_=pos_d[0:1, t0:t0 + T].partition_broadcast(128)),
                       "posi", writes=[("posi", "*")])
                ta, tb, tcc = ntmp(), ntmp(), ntmp()
                ka, kb, kcx = ("tmp%d" % ta, "*"), ("tmp%d" % tb, "*"), ("tmp%d" % tcc, "*")
                sc.op("dve", lambda e, ta=ta: e.tensor_copy(out=tmp[ta][:], in_=posi[:]), reads=[("posi", "*")], writes=[ka])
                sc.op("dve", lambda e, ta=ta: e.tensor_scalar(out=tmp[ta][:], in0=tmp[ta][:], scalar1=C("freq"),
                                                              scalar2=None, op0=ALU.mult),
                      reads=[ka, ("cp", "*")], writes=[ka])
                for which, dstt, dkey in ((0, ropeS, ("ropeS", "*")), (1, ropeC, ("ropeC", "*"))):
                    if which == 1:
                        sc.op("dve", lambda e, ta=ta: e.tensor_scalar(out=tmp[ta][:], in0=tmp[ta][:], scalar1=math.pi / 2,
                                                                      scalar2=None, op0=ALU.add), reads=[ka], writes=[ka])
                    sc.op("dve", lambda e, ta=ta: e.tensor_scalar(out=posi[:], in0=tmp[ta][:], scalar1=1.0 / (2 * math.pi),
                                                                  scalar2=None, op0=ALU.mult),
                          reads=[ka], writes=[("posi", "*")])
                    sc.op("dve", lambda e, tb=tb: e.tensor_copy(out=tmp[tb][:], in_=posi[:]), reads=[("posi", "*")],
                          writes=[kb])
                    sc.op("dve", lambda e, ta=ta, tb=tb: e.scalar_tensor_tensor(
                        out=tmp[tb][:], in0=tmp[tb][:], scalar=-2 * math.pi, in1=tmp[ta][:], op0=ALU.mult, op1=ALU.add),
                        reads=[ka, kb], writes=[kb])
                    sc.op("dve", lambda e, tb=tb, tcc=tcc: e.tensor_scalar(out=tmp[tcc][:], in0=tmp[tb][:], scalar1=math.pi,
                                                                          scalar2=2 * math.pi, op0=ALU.is_gt, op1=ALU.mult),
                          reads=[kb], writes=[kcx])
                    sc.op("dve", lambda e, tb=tb, tcc=tcc: e.tensor_tensor(out=tmp[tb][:], in0=tmp[tb][:], in1=tmp[tcc][:],
                                                                          op=ALU.subtract), reads=[kb, kcx], writes=[kb])
                    sc.op("act", lambda e, tb=tb, dstt=dstt: e.activation(out=dstt[:], in_=tmp[tb][:], func=AF.Sin),
                          reads=[kb], writes=[dkey])
                sc.op("dve", lambda e: e.tensor_scalar(out=ropeS[:], in0=ropeS[:], scalar1=C("sgn"), scalar2=None,
                                                       op0=ALU.mult), reads=[("ropeS", "*"), ("cp", "*")],
                      writes=[("ropeS", "*")])

            if it == 0:
                rope_tables(0)
            sc.op("dve", lambda e: e.tensor_copy(out=uT[:, :, 0:30], in_=uT[:, :, T:T + 30]), reads=[("uT", "*")],
                  writes=[("uT", "*")])
            rmsnorm("g_mix", T)
            for blk in range(8):
                s = wblock(wfm_d, blk * 256, 256)
                bA, bB = nbank(), nbank()
                proj(s, 0, T, bA)
                proj(s, 128, T, bB)
                t1, t2 = ntmp(), ntmp()
                k1, k2 = ("tmp%d" % t1, "*"), ("tmp%d" % t2, "*")
                sc.op("dve", lambda e, t1=t1, bA=bA: e.tensor_tensor(out=tmp[t1][:], in0=bank(bA), in1=ropeC[:],
                                                                    op=ALU.mult),
                      reads=[bkey(bA), ("ropeC", "*")], writes=[k1])
                sc.op("dve", lambda e, t2=t2, bB=bB: e.tensor_tensor(out=tmp[t2][:], in0=bank(bB), in1=ropeS[:],
                                                                    op=ALU.mult),
                      reads=[bkey(bB), ("ropeS", "*")], writes=[k2])
                if blk < 6:
                    dv, dk = BB[:, blk, :], ("BB", blk)
                elif blk == 6:
                    dv, dk = kT2[:, t0:t0 + T], ("kT2", it)
                else:
                    dv, dk = kiT2[:, t0:t0 + T], ("kiT2", it)
                sc.op("dve", lambda e, t1=t1, t2=t2, dv=dv: e.tensor_tensor(out=dv, in0=tmp[t1][:], in1=tmp[t2][:],
                                                                           op=ALU.add),
                      reads=[k1, k2], writes=[dk])
            for c in range(4):
                s = wblock(wfm_d, 2048 + c * 256, 256)
                bA, bB = nbank(), nbank()
                proj(s, 0, T, bA)
                proj(s, 128, T, bB)
                t1 = ntmp()
                k1 = ("tmp%d" % t1, "*")
                sc.op("act", lambda e, t1=t1, bB=bB: e.activation(out=tmp[t1][:], in_=bank(bB), func=AF.Tanh, scale=0.5),
                      reads=[bkey(bB)], writes=[k1])
                sc.op("dve", lambda e, t1=t1, bA=bA, c=c: e.scalar_tensor_tensor(
                    out=uT[:, c, 30:30 + T], in0=tmp[t1][:], scalar=1.0, in1=bank(bA), op0=ALU.add, op1=ALU.mult),
                    reads=[k1, bkey(bA)], writes=[("uT", "*")])
            for s4 in range(4):
                b = nbank()
                for kc in range(KC):
                    sc.op("pe", lambda e, b=b, kc=kc, s4=s4: e.matmul(bank(b)[:, 0:68], hT[:, kc, s4 * 128:(s4 + 1) * 128],
                                                                     wvw[:, kc, :], start=(kc == 0), stop=(kc == KC - 1)),
                          reads=[("wvw", "*"), ("hT", kc)], writes=[bkey(b)])
                qi_ = it * 4 + s4
                sc.op("act", lambda e, b=b, qi_=qi_: e.activation(out=vaug[:, qi_, 0:64], in_=bank(b)[:, 0:64],
                                                                  func=AF.Copy), reads=[bkey(b)], writes=[("vaug", qi_), ("vwtok", "*")])
                sc.op("dve", lambda e, b=b, s4=s4: e.tensor_scalar(out=wis[:, s4, :], in0=bank(b)[:, 64:68],
                                                                   scalar1=0.5 * 0.125, scalar2=None, op0=ALU.mult),
                      reads=[bkey(b), ("vwtok", "*")], writes=[("wis", s4)])

            if it + 1 < NT:
                x_load(it + 1, 0)
                x_load(it + 1, 1)

            def dsa_index(s4):
                j = it * 4 + s4
                n = (j + 1) * 128
                nck = (n + 511) // 512
                for ck in range(nck):
                    w = min(512, n - ck * 512)
                    for h in range(4):
                        b = nbank()
                        p0 = (h % 2) * 64
                        sc.op("pe", lambda e, b=b, h=h, p0=p0, ck=ck, w=w, s4=s4: e.matmul(
                            bank(b)[:, 0:w], BB[p0:p0 + 64, 4 + h // 2, s4 * 128:(s4 + 1) * 128],
                            kiT2[p0:p0 + 64, ck * 512:ck * 512 + w], start=True, stop=True),
                            reads=[("BB", 4 + h // 2), ("kiT2", "*")], writes=[bkey(b)])
                        r = rtmp[h % 2]
                        rk = ("rtmp%d" % (h % 2), "*")
                        sc.op("act", lambda e, b=b, r=r, w=w: e.activation(out=r[:, 0:w], in_=bank(b)[:, 0:w],
                                                                           func=AF.Relu), reads=[bkey(b)], writes=[rk])
                        sv = scores[:, ck * 512:ck * 512 + w]
                        if h == 0:
                            sc.op("dve", lambda e, r=r, w=w, sv=sv, s4=s4: e.tensor_scalar(
                                out=sv, in0=r[:, 0:w], scalar1=wis[:, s4, 0:1], scalar2=None, op0=ALU.mult),
                                reads=[rk, ("wis", s4)], writes=[("scores", "*")])
                        else:
                            sc.op("dve", lambda e, r=r, w=w, sv=sv, s4=s4, h=h: e.scalar_tensor_tensor(
                                out=sv, in0=r[:, 0:w], scalar=wis[:, s4, h:h + 1], in1=sv, op0=ALU.mult, op1=ALU.add),
                                reads=[rk, ("wis", s4), ("scores", "*")], writes=[("scores", "*")])
                sc.op("dve", lambda e, j=j: e.tensor_tensor(out=scores[:, j * 128:(j + 1) * 128],
                                                            in0=scores[:, j * 128:(j + 1) * 128], in1=causal,
                                                            op=ALU.add),
                      reads=[("scores", "*"), ("cmf", "*")], writes=[("scores", "*")])

            def dsa_bisect(s4):
                j = it * 4 + s4
                n = (j + 1) * 128
                nA = ((n * 7 // 20) // 128) * 128
                maskb = maskbs[j % 2]
                mk = ("maskb%d" % (j % 2), "*")
                junkA = prodT[:].rearrange("p a b -> p (a b)")
                sc.op("dve", lambda e: e.memset(bis[:, 2:3], 2.0 ** -13), writes=[("bis", 2)])
                for i in range(NBIS):
                    dl = 8.0 / (2 ** i)
                    nxt = dl / 2 if i < NBIS - 1 else dl
                    if nA > 0:
                        sc.op("act", lambda e, nA=nA: e.activation(
                            out=junkA[:, 0:nA], in_=scores[:, 0:nA], func=AF.Sign, bias=bis[:, 2:3], scale=-1.0,
                            accum_out=bis[:, 3:4]),
                            reads=[("scores", "*"), ("bis", 2)], writes=[("prodT", "*"), ("bis", 3)])
                    sc.op("dve", lambda e, n=n, nA=nA, maskb=maskb: e.tensor_scalar(
                        out=maskb[:, nA:n], in0=scores[:, nA:n], scalar1=bis[:, 2:3], scalar2=0.0, op0=ALU.is_gt,
                        op1=ALU.add, accum_out=bis[:, 0:1]),
                        reads=[("scores", "*"), ("bis", 2)], writes=[mk, ("bis", 0)])
                    if nA > 0:
                        sc.op("dve", lambda e: e.scalar_tensor_tensor(
                            out=bis[:, 1:2], in0=bis[:, 0:1], scalar=2.0, in1=bis[:, 3:4], op0=ALU.mult,
                            op1=ALU.subtract), reads=[("bis", 0), ("bis", 3)], writes=[("bis", 1)])
                        sc.op("dve", lambda e, dl=dl, nA=nA: e.tensor_scalar(
                            out=bis[:, 1:2], in0=bis[:, 1:2], scalar1=2.0 * TOPK - 1.0 - nA, scalar2=dl, op0=ALU.is_ge,
                            op1=ALU.mult), reads=[("bis", 1)], writes=[("bis", 1)])
                    else:
                        sc.op("dve", lambda e, dl=dl: e.tensor_scalar(
                            out=bis[:, 1:2], in0=bis[:, 0:1], scalar1=TOPK - 0.5, scalar2=dl, op0=ALU.is_ge,
                            op1=ALU.mult), reads=[("bis", 0)], writes=[("bis", 1)])
                    sc.op("dve", lambda e, nxt=nxt: e.scalar_tensor_tensor(out=bis[:, 2:3], in0=bis[:, 1:2], scalar=-nxt,
                                                                           in1=bis[:, 2:3], op0=ALU.add, op1=ALU.add),
                          reads=[("bis", 1), ("bis", 2)], writes=[("bis", 2)])
                    yield
                sc.op("dve", lambda e, n=n, maskb=maskb: e.tensor_scalar(
                    out=maskb[:, 0:n], in0=scores[:, 0:n], scalar1=bis[:, 2:3], scalar2=NEG, op0=ALU.is_le,
                    op1=ALU.mult), reads=[("scores", "*"), ("bis", 2)], writes=[mk])
                yield

            def dsa_attend(s4):
                j = it * 4 + s4
                maskb = maskbs[j % 2]
                mk = ("maskb%d" % (j % 2), "*")
                def att_L(c):
                    pr = npair()
                    for par in range(2):
                        p0 = par * 64
                        ov = ps[pr][:, par * 512:(par + 1) * 512]
                        sc.op("pe", lambda e, ov=ov, p0=p0, c=c, s4=s4: e.matmul(
                            ov.rearrange("p (a b) -> p a b", a=4), kT2[p0:p0 + 64, c * 128:(c + 1) * 128],
                            BB[p0:p0 + 64, 0:4, s4 * 128:(s4 + 1) * 128], start=True, stop=False),
                            reads=[("kT2", "*"), ("BB", 0), ("BB", 1), ("BB", 2), ("BB", 3)],
                            writes=[bkey(2 * pr + par)])
                    for par in range(2):
                        ov = ps[pr][:, par * 512:(par + 1) * 512]
                        sc.op("pe", lambda e, ov=ov, c=c, maskb=maskb: e.matmul(
                            ov, maskb[:, c * 128:(c + 1) * 128], E4, start=False, stop=True),
                            reads=[mk, ("cmb", "*")], writes=[bkey(2 * pr + par)])
                    pt = PT[c % 2]
                    pk = ("PT%d" % (c % 2), "*")
                    sc.op("act", lambda e, pr=pr, pt=pt: e.activation(out=pt[:], in_=ps[pr][:], func=AF.Exp, scale=0.125),
                          reads=[bkey(2 * pr), bkey(2 * pr + 1)], writes=[pk])

                def att_PV(c):
                    pt = PT[c % 2]
                    pk = ("PT%d" % (c % 2), "*")
                    for par in range(2):
                        sc.op("pe", lambda e, par=par, pt=pt, c=c, j=j: e.matmul(
                            ps[3][:, par * 512:(par + 1) * 512], vaug[:, c, :], pt[:, par * 512:(par + 1) * 512],
                            start=(c == 0), stop=(c == j)), reads=[pk, ("vaug", c)], writes=[bkey(6 + par)])

                att_L(0)
                yield
                for c in range(1, j + 1):
                    att_L(c)
                    att_PV(c - 1)
                    yield
                att_PV(j)
                sc.op("act", lambda e: e.activation(out=rec[64:128, :], in_=ps[3][64:128, :], func=AF.Ln),
                      reads=[bkey(6), bkey(7)], writes=[("rec", "*")])
                sc.op("act", lambda e: e.activation(out=rec[64:128, :], in_=rec[64:128, :], func=AF.Exp, scale=-1.0),
                      reads=[("rec", "*")], writes=[("rec", "*")])
                for par in range(2):
                    sc.op("dve", lambda e, par=par, s4=s4: e.tensor_tensor(
                        out=BC[par * 64:(par + 1) * 64, 0:4, s4 * 128:(s4 + 1) * 128],
                        in0=ps[3][0:64, par * 512:(par + 1) * 512].rearrange("p (a b) -> p a b", a=4),
                        in1=rec[64:128, par * 512:(par + 1) * 512].rearrange("p (a b) -> p a b", a=4), op=ALU.mult),
                        reads=[("rec", "*"), bkey(6 + par)], writes=[("BC", a_) for a_ in range(4)])

            def run_pair(ga, na, gb, nb):
                ia = ib = 0
                da = db = False
                while not (da and db):
                    take_b = (not db) and (da or ib * na <= ia * nb)
                    try:
                        next(gb if take_b else ga)
                    except StopIteration:
                        if take_b:
                            db = True
                        else:
                            da = True
                    else:
                        if take_b:
                            ib += 1
                        else:
                            ia += 1

            st["wide"] = False
            dsa_index(0)
            for _ in dsa_bisect(0):
                pass
            for s4 in range(1, 4):
                dsa_index(s4)
                run_pair(dsa_bisect(s4), NBIS + 1, dsa_attend(s4 - 1), it * 4 + s4 + 1)
            for _ in dsa_attend(3):
                pass
            st["wide"] = True

            yv = scores[:, 0:2048].rearrange("p (c t) -> p c t", c=4)
            bS, bQ = nbank(), nbank()
            for c in range(4):
                b = nbank()
                for jt in range(31):
                    pv = pcv[jt % 4]
                    pk = ("pcv%d" % (jt % 4), "*")
                    if jt % 3 == 0:
                        sc.op("act", lambda e, pv=pv, c=c, jt=jt: e.activation(
                            out=pv[:], in_=uT[:, c, jt:jt + T], func=AF.Copy, scale=cbw(c, jt)),
                            reads=[("uT", "*"), ("cder", "*")], writes=[pk])
                    else:
                        sc.op("dve", lambda e, pv=pv, c=c, jt=jt: e.tensor_scalar(
                            out=pv[:], in0=uT[:, c, jt:jt + T], scalar1=cbw(c, jt), scalar2=None, op0=ALU.mult),
                            reads=[("uT", "*"), ("cder", "*")], writes=[pk])
                    sc.op("pe", lambda e, b=b, pv=pv, jt=jt: e.matmul(bank(b), E4[:, 0:128], pv[:], start=(jt == 0),
                                                                     stop=(jt == 30)),
                          reads=[pk, ("cmb", "*")], writes=[bkey(b)])
                tb = ntmp()
                kb = ("tmp%d" % tb, "*")
                sc.op("act", lambda e, b=b, c=c: e.activation(out=yv[:, c, :], in_=bank(b), func=AF.Identity,
                                                              bias=C("cb_b", c)),
                      reads=[bkey(b), ("cp", "*")], writes=[("scores", "*")])
                sc.op("act", lambda e, tb=tb, c=c: e.activation(out=tmp[tb][:], in_=yv[:, c, :], func=AF.Square),
                      reads=[("scores", "*")], writes=[kb])
                sc.op("pe", lambda e, c=c, bS=bS: e.matmul(bank(bS), ones_f, yv[:, c, :], start=(c == 0), stop=(c == 3)),
                      reads=[("scores", "*"), ("cmf", "*")], writes=[bkey(bS)])
                sc.op("pe", lambda e, c=c, bQ=bQ, tb=tb: e.matmul(bank(bQ), ones_f, tmp[tb][:], start=(c == 0),
                                                                 stop=(c == 3)),
                      reads=[kb, ("cmf", "*")], writes=[bkey(bQ)])
            tm, tv, t2 = ntmp(), ntmp(), ntmp()
            km, kv, k2 = ("tmp%d" % tm, "*"), ("tmp%d" % tv, "*"), ("tmp%d" % t2, "*")
            sc.op("act", lambda e, tm=tm, bS=bS: e.activation(out=tmp[tm][:], in_=bank(bS), func=AF.Copy, scale=1.0 / 512),
                  reads=[bkey(bS)], writes=[km])
            sc.op("dve", lambda e, tm=tm, t2=t2: e.tensor_tensor(out=tmp[t2][:], in0=tmp[tm][:], in1=tmp[tm][:],
                                                                op=ALU.mult), reads=[km], writes=[k2])
            sc.op("dve", lambda e, tv=tv, bQ=bQ: e.tensor_scalar(out=tmp[tv][:], in0=bank(bQ), scalar1=1.0 / 512,
                                                                scalar2=EPS, op0=ALU.mult, op1=ALU.add),
                  reads=[bkey(bQ)], writes=[kv])
            sc.op("dve", lambda e, tv=tv, t2=t2: e.tensor_tensor(out=tmp[tv][:], in0=tmp[tv][:], in1=tmp[t2][:],
                                                                op=ALU.subtract), reads=[kv, k2], writes=[kv])
            sc.op("act", lambda e, tv=tv: e.activation(out=tmp[tv][:], in_=tmp[tv][:], func=AF.Ln),
                  reads=[kv], writes=[kv])
            sc.op("act", lambda e, tv=tv: e.activation(out=tmp[tv][:], in_=tmp[tv][:], func=AF.Exp, scale=-0.5),
                  reads=[kv], writes=[kv])
            for c in range(4):
                ta, tb = ntmp(), ntmp()
                while ta in (tm, tv) or tb in (tm, tv) or ta == tb:
                    ta, tb = ntmp(), ntmp()
                ka, kb = ("tmp%d" % ta, "*"), ("tmp%d" % tb, "*")
                sc.op("dve", lambda e, ta=ta, c=c, tm=tm: e.tensor_tensor(out=tmp[ta][:], in0=yv[:, c, :], in1=tmp[tm][:],
                                                                         op=ALU.subtract),
                      reads=[("scores", "*"), km], writes=[ka])
                sc.op("dve", lambda e, ta=ta, tv=tv: e.tensor_tensor(out=tmp[ta][:], in0=tmp[ta][:], in1=tmp[tv][:],
                                                                    op=ALU.mult), reads=[ka, kv], writes=[ka])
                sc.op("dve", lambda e, ta=ta, c=c: e.tensor_scalar(out=tmp[ta][:], in0=tmp[ta][:], scalar1=lng(c),
                                                                  scalar2=lnb(c), op0=ALU.mult, op1=ALU.add),
                      reads=[ka, ("cder", "*")], writes=[ka])
                sc.op("act", lambda e, ta=ta, tb=tb: e.activation(out=tmp[tb][:], in_=tmp[ta][:], func=AF.Tanh),
                      reads=[ka], writes=[kb])
                sc.op("dve", lambda e, ta=ta, tb=tb, c=c: e.scalar_tensor_tensor(
                    out=BC[:, 4 + c, :], in0=tmp[tb][:], scalar=1.0, in1=tmp[ta][:], op0=ALU.add, op1=ALU.mult),
                    reads=[ka, kb], writes=[("BC", 4 + c)])
            for dc in range(8):
                s = wblock(wout_d, dc * 128, 128)
                b = nbank()
                proj(s, 0, T, b, rhs_fn=lambda kc: BC[:, kc, :], rkeys=lambda kc: ("BC", kc), ncols=128)
                sc.op("dve", lambda e, b=b, dc=dc: e.tensor_tensor(out=xT[:, dc, :], in0=xT[:, dc, :], in1=bank(b),
                                                                  op=ALU.add),
                      reads=[bkey(b), ("xT", dc)], writes=[("xT", dc)])

            rmsnorm("g_cross", T)
            for blk in range(4):
                s = wblock(wq_d, blk * 256, 256)
                for cc in range(2):
                    b = nbank()
                    proj(s, cc * 128, T, b)
                    c = blk * 2 + cc
                    sc.op("act", lambda e, b=b, c=c: e.activation(out=BB[:, c, :], in_=bank(b), func=AF.Copy),
                          reads=[bkey(b)], writes=[("BB", c)])
            def cross_L(h):
                pts = []
                for mc in range(2):
                    b = nbank()
                    for kk in range(2):
                        sc.op("pe", lambda e, b=b, h=h, kk=kk, mc=mc: e.matmul(
                            bank(b), kcT[:, 2 * h + kk, mc * 128:(mc + 1) * 128], BB[:, 2 * h + kk, :], start=(kk == 0),
                            stop=(kk == 1)), reads=[("kcT", 2 * h + kk), ("BB", 2 * h + kk)], writes=[bkey(b)])
                    pt = PT[mc][:, (h % 2) * 512:(h % 2 + 1) * 512]
                    pk = ("PT%d" % mc, h % 2)
                    sc.op("act", lambda e, b=b, pt=pt: e.activation(out=pt, in_=bank(b), func=AF.Exp, scale=1.0 / 16),
                          reads=[bkey(b)], writes=[pk])
                    pts.append((pt, pk))
                return pts

            def cross_D(h, pts):
                bD = nbank()
                for mc in range(2):
                    sc.op("pe", lambda e, bD=bD, mc=mc, pt=pts[mc][0]: e.matmul(bank(bD), ones_b, pt, start=(mc == 0),
                                                                              stop=(mc == 1)),
                          reads=[pts[mc][1], ("cmb", "*")], writes=[bkey(bD)])
                tr = ntmp()
                kr = ("tmp%d" % tr, "*")
                sc.op("act", lambda e, tr=tr, bD=bD: e.activation(out=tmp[tr][:], in_=bank(bD), func=AF.Ln),
                      reads=[bkey(bD)], writes=[kr])
                sc.op("act", lambda e, tr=tr: e.activation(out=tmp[tr][:], in_=tmp[tr][:], func=AF.Exp, scale=-1.0),
                      reads=[kr], writes=[kr])
                for dd in range(2):
                    b = nbank()
                    cdx = 2 * h + dd
                    for mc in range(2):
                        sc.op("pe", lambda e, b=b, mc=mc, cdx=cdx, pt=pts[mc][0]: e.matmul(
                            bank(b), vc[:, mc, cdx * 128:(cdx + 1) * 128], pt, start=(mc == 0), stop=(mc == 1)),
                            reads=[pts[mc][1], ("vc", "*")], writes=[bkey(b)])
                    sc.op("dve", lambda e, b=b, cdx=cdx, tr=tr: e.tensor_tensor(out=BC[:, cdx, :], in0=bank(b),
                                                                                in1=tmp[tr][:], op=ALU.mult),
                          reads=[bkey(b), kr], writes=[("BC", cdx)])

            prev_pts = cross_L(0)
            for h in range(1, 4):
                cur_pts = cross_L(h)
                cross_D(h - 1, prev_pts)
                prev_pts = cur_pts
            cross_D(3, prev_pts)
            for blk in range(4):
                s = wblock(wo_d, blk * 256, 256)
                for cc in range(2):
                    b = nbank()
                    dc = blk * 2 + cc
                    proj(s, cc * 128, T, b, rhs_fn=lambda kc: BC[:, kc, :], rkeys=lambda kc: ("BC", kc))
                    sc.op("dve", lambda e, b=b, dc=dc: e.tensor_tensor(out=xT[:, dc, :], in0=xT[:, dc, :], in1=bank(b),
                                                                      op=ALU.add),
                          reads=[bkey(b), ("xT", dc)], writes=[("xT", dc)])

            rmsnorm("g_ffn", T)
            if it + 1 < NT:
                rope_tables(it + 1)
            for g in range(NF // FG):
                def ffn_A1(fl):
                    f = g * FG + fl
                    srcG = wg_d[:, f * 128:(f + 1) * 128].rearrange("(kc p) n -> p kc n", p=128)
                    srcU = wu_d[:, f * 128:(f + 1) * 128].rearrange("(kc p) n -> p kc n", p=128)
                    s = wload([(lambda r: r[:, 0:1024].rearrange("p (kc n) -> p kc n", kc=8), srcG),
                               (lambda r: r[:, 1024:2048].rearrange("p (kc n) -> p kc n", kc=8), srcU)])
                    bG, bU = nbank(), nbank()
                    proj(s, 0, T, bG, ncols=128, off=0)
                    proj(s, 0, T, bU, ncols=128, off=1024)
                    gs = Gs[f % 2]
                    gk = ("Gs%d" % (f % 2), "*")
                    sc.op("dve", lambda e, gs=gs, f=f: e.tensor_copy(out=gs[:, 0:2], in_=halo[:, f, :]),
                          reads=[("halo", f)], writes=[gk])
                    sc.op("act", lambda e, gs=gs, bG=bG: e.activation(out=gs[:, 2:2 + T], in_=bank(bG), func=AF.Copy),
                          reads=[bkey(bG)], writes=[gk])
                    ta, tb = ntmp(), ntmp()
                    ka, kb = ("tmp%d" % ta, "*"), ("tmp%d" % tb, "*")
                    fw = lambda jj, f=f: cp[:, coff["f_w"] + f * 3 + jj:coff["f_w"] + f * 3 + jj + 1]
                    sc.op("act", lambda e, ta=ta, bG=bG, f=f, fw=fw: e.activation(
                        out=tmp[ta][:], in_=bank(bG), func=AF.Identity, scale=fw(2), bias=C("f_b", f)),
                        reads=[bkey(bG), ("cp", "*")], writes=[ka])
                    sc.op("act", lambda e, gs=gs, f=f: e.activation(out=halo[:, f, :], in_=gs[:, T:T + 2], func=AF.Copy),
                          reads=[gk], writes=[("halo", f)])
                    return (ta, tb, ka, kb, bU, gs, gk, fw)

                def ffn_A2(stt_):
                    ta, tb, ka, kb, bU, gs, gk, fw = stt_
                    for jj in (1, 0):
                        sc.op("dve", lambda e, ta=ta, gs=gs, jj=jj, fw=fw: e.scalar_tensor_tensor(
                            out=tmp[ta][:], in0=gs[:, jj:jj + T], scalar=fw(jj), in1=tmp[ta][:], op0=ALU.mult,
                            op1=ALU.add), reads=[gk, ("cp", "*"), ka], writes=[ka])
                    sc.op("act", lambda e, ta=ta, tb=tb: e.activation(out=tmp[tb][:], in_=tmp[ta][:], func=AF.Tanh,
                                                                      scale=0.5), reads=[ka], writes=[kb])

                def ffn_B(fl, stt_):
                    ta, tb, ka, kb, bU = stt_[:5]
                    sc.op("dve", lambda e, ta=ta, tb=tb: e.scalar_tensor_tensor(
                        out=tmp[tb][:], in0=tmp[tb][:], scalar=1.0, in1=tmp[ta][:], op0=ALU.add, op1=ALU.mult),
                        reads=[ka, kb], writes=[kb])
                    sc.op("dve", lambda e, tb=tb, bU=bU, fl=fl: e.tensor_tensor(out=prodT[:, fl, :], in0=tmp[tb][:],
                                                                               in1=bank(bU), op=ALU.mult),
                          reads=[kb, bkey(bU)], writes=[("prodT", fl)])

                sts_ = {}
                for step in range(FG + 2):
                    if step < FG:
                        sts_[step] = ffn_A1(step)
                    if 0 <= step - 1 < FG:
                        ffn_A2(sts_[step - 1])
                    if 0 <= step - 2 < FG:
                        ffn_B(step - 2, sts_[step - 2])
                for dc in range(8):
                    src = wd_d[g * FG * 128:(g + 1) * FG * 128, dc * 128:(dc + 1) * 128].rearrange(
                        "(kc p) n -> p kc n", p=128)
                    s = wload([(lambda r: r[:, 0:FG * 128].rearrange("p (kc n) -> p kc n", kc=FG), src)])
                    b = nbank()
                    proj(s, 0, T, b, rhs_fn=lambda kc: prodT[:, kc, :], rkeys=lambda kc: ("prodT", kc), nk=FG, ncols=128)
                    sc.op("dve", lambda e, b=b, dc=dc: e.scalar_tensor_tensor(
                        out=xT[:, dc, :], in0=bank(b), scalar=0.5, in1=xT[:, dc, :], op0=ALU.mult, op1=ALU.add),
                        reads=[bkey(b), ("xT", dc)], writes=[("xT", dc)])

            rmsnorm("g_fin", T, inplace=True)
            for s4 in range(4):
                for half in range(2):
                    b = nbank()
                    for i in range(4):
                        dc = half * 4 + i
                        sc.op("pe", lambda e, b=b, i=i, dc=dc, s4=s4: e.transpose(
                            out=bank(b)[:, i * 128:(i + 1) * 128], in_=xT[:, dc, s4 * 128:(s4 + 1) * 128],
                            identity=ident), reads=[("xT", dc), ("cmf", "*")], writes=[bkey(b)])
                    if half == 0:
                        sc.op("act", lambda e, b=b, s4=s4: e.activation(out=osts[s4 % 2][:, 0:512], in_=bank(b),
                                                                        func=AF.Copy),
                              reads=[bkey(b)], writes=[("ost%d" % (s4 % 2), 0)])
                    else:
                        sc.op("dve", lambda e, b=b, s4=s4: e.tensor_copy(out=osts[s4 % 2][:, 512:1024], in_=bank(b)),
                              reads=[bkey(b)], writes=[("ost%d" % (s4 % 2), 1)])
                sc.dma("sp", lambda e, r0=t0 + s4 * 128, s4=s4: e.dma_start(out=out_d[r0:r0 + 128, :],
                                                                            in_=osts[s4 % 2][:]),
                       "ost%d" % (s4 % 2), reads=[("ost%d" % (s4 % 2), "*")])
        sc.op("sp", None, writes=[("ost0", "*"), ("ost1", "*")])
        block = es.enter_context(nc.Block())
        sc.emit(block)
    return nc


def _swap_cols(n_heads):
    idx = []
    for h in range(n_heads):
        base = h * 64
        idx += [base + 8 + i for i in range(8)] + [base + i for i in range(8)] + [base + i for i in range(16, 64)]
    return np.array(idx)


def host_layout(inputs):
    f = lambda k: np.asarray(inputs[k], dtype=np.float32)
    w_in = f("w_in")[0]
    q, k, v, qi, ki, wi, glu = np.split(w_in, np.cumsum([512, 64, 64, 256, 64, 4, 1024])[:-1], axis=1)
    base = np.concatenate([q, qi, k, k, ki, ki], axis=1)
    sw = base[:, _swap_cols(16)]
    ga, gg = glu[:, :512], glu[:, 512:]
    blocks = []
    for c in range(8):
        blocks += [base[:, c * 128:(c + 1) * 128], sw[:, c * 128:(c + 1) * 128]]
    for c in range(4):
        blocks += [ga[:, c * 128:(c + 1) * 128], gg[:, c * 128:(c + 1) * 128]]
    w_fm = np.ascontiguousarray(np.concatenate(blocks, axis=1))
    w_vw = np.ascontiguousarray(np.concatenate([v, wi], axis=1))
    coff, ncp = cpack_layout()
    cpk = np.zeros((128, ncp), np.float32)
    col = lambda vec, n: vec.reshape(n, 128).T
    cpk[:, coff["g_mix"]:coff["g_mix"] + 8] = col(f("norm_mix_g")[0], 8)
    cpk[:, coff["g_cross"]:coff["g_cross"] + 8] = col(f("norm_cross_g")[0], 8)
    cpk[:, coff["g_mem"]:coff["g_mem"] + 8] = col(f("norm_mem_g")[0], 8)
    cpk[:, coff["g_ffn"]:coff["g_ffn"] + 8] = col(f("norm_ffn_g")[0], 8)
    cpk[:, coff["g_fin"]:coff["g_fin"] + 8] = col(f("norm_final_g"), 8)
    cpk[:, coff["ln_g"]:coff["ln_g"] + 4] = col(f("ln_b_g")[0], 4)
    cpk[:, coff["ln_b"]:coff["ln_b"] + 4] = col(f("ln_b_b")[0], 4)
    cpk[:, coff["cb_b"]:coff["cb_b"] + 4] = col(f("conv_b_b")[0], 4)
    cw = f("conv_b_w")[0]
    cpk[:, coff["cb_w"]:coff["cb_w"] + 124] = cw.reshape(31, 4, 128).transpose(2, 1, 0).reshape(128, 124)
    fw = f("ffn_conv_w")[0]
    cpk[:, coff["f_w"]:coff["f_w"] + NF * 3] = fw.reshape(3, NF, 128).transpose(2, 1, 0).reshape(128, NF * 3)
    cpk[:, coff["f_b"]:coff["f_b"] + NF] = col(f("ffn_conv_b")[0], NF)
    p = np.arange(128) % 64
    freqs = (np.float32(500000.0) ** (-np.arange(0, 16, 2, dtype=np.float32) / np.float32(16))).astype(np.float32)
    cpk[:, coff["freq"]] = np.where(p < 16, freqs[p % 8], 0.0)
    cpk[:, coff["sgn"]] = np.where(p < 8, -1.0, np.where(p < 16, 1.0, 0.0))
    cm = np.zeros((128, 896), np.float32)
    cm[:, 0:128] = np.eye(128)
    qq, kk = np.meshgrid(np.arange(128), np.arange(128), indexing="ij")
    cm[:, 128:256] = np.where(kk <= qq, 0.0, -1e30)
    cm[:, 256:384] = 1.0
    cm[:, 384:896] = np.tile(np.eye(128, dtype=np.float32), (1, 4))
    shared = {
        "w_fm": w_fm, "w_vw": w_vw, "w_out": f("w_out")[0], "w_q": f("w_q_cross")[0], "w_k": f("w_k_cross")[0],
        "w_v": f("w_v_cross")[0], "w_o": f("w_o_cross")[0], "w_gate": f("w_gate")[0], "w_up": f("w_up")[0],
        "w_down": f("w_down")[0], "cpack": cpk, "cmats": cm,
    }
    return shared


_NC_CACHE = {}


def kernel(**inputs):
    x = np.asarray(inputs["x"], dtype=np.float32)
    mem = np.asarray(inputs["mem"], dtype=np.float32)
    pos = np.asarray(inputs["positions"], dtype=np.int32)
    B, S, _ = x.shape
    shared = host_layout(inputs)
    if S not in _NC_CACHE:
        _NC_CACHE[S] = build(S)
    nc = _NC_CACHE[S]
    in_maps = []
    for b in range(B):
        m = dict(shared)
        m["x"] = np.ascontiguousarray(x[b])
        m["mem"] = np.ascontiguousarray(mem[b])
        m["pos"] = np.ascontiguousarray(pos[b][None, :])
        in_maps.append(m)
    res = run_bass_kernel_spmd(nc, in_maps, core_ids=list(range(B)))
    return np.stack([np.asarray(r["out"], dtype=np.float32) for r in res.results], axis=0)
```
